# Optimizing a Trainium2 kernel written in Bass

```python
import jax, jax.numpy as jnp
from jax import lax
import numpy as np

D_MODEL = 1024
BATCH = 4
SEQ = 8192
DEPTH = 2

GRID_W = 64
CTX_LEN = 256
EPS = 1e-6

D_CONV = 256
CONV_GROUPS = 4
CONV_W = 3
D_SGU = 256
SGU_HEADS = 4
SGU_HEAD_DIM = D_SGU // SGU_HEADS
SGU_CHUNK = 128
N_HEADS = 8
N_KV_HEADS = 2
HEAD_DIM = 64
Q_PER_KV = N_HEADS // N_KV_HEADS
D_ATTN = N_HEADS * HEAD_DIM
WINDOW = 128
BLOCK = 128
ROPE_THETA = 10000.0
ROPE_AXIS_DIM = HEAD_DIM // 2
ROPE_FREQS = ROPE_AXIS_DIM // 2
D_MIX = D_CONV + D_SGU + D_ATTN

CONV_END = 3 * D_CONV
SGU_END = CONV_END + 2 * D_SGU
Q_END = SGU_END + D_ATTN
K_END = Q_END + N_KV_HEADS * HEAD_DIM
D_IN = K_END + N_KV_HEADS * HEAD_DIM

N_KEYS = 128
N_EXPERTS = N_KEYS * N_KEYS
PEER_HEADS = 8
PEER_TOPK = 16
PEER_DQ = 256
PEER_DHALF = PEER_DQ // 2
EXPERT_CHUNK = 128

kernel_name = "hybrid_parallel_heads_peer_dit"


def rmsnorm(x, g):
    xf = x.astype(jnp.float32)
    y = xf * lax.rsqrt(jnp.mean(xf * xf, axis=-1, keepdims=True) + EPS)
    return (y * g.astype(jnp.float32)).astype(x.dtype)


def rms_plain(x):
    xf = x.astype(jnp.float32)
    return (xf * lax.rsqrt(jnp.mean(xf * xf, axis=-1, keepdims=True) + EPS)).astype(x.dtype)


def layernorm(x, g):
    xf = x.astype(jnp.float32)
    mu = jnp.mean(xf, axis=-1, keepdims=True)
    var = jnp.mean(jnp.square(xf - mu), axis=-1, keepdims=True)
    return ((xf - mu) * lax.rsqrt(var + EPS) * g.astype(jnp.float32)).astype(x.dtype)


def axial_rope_tables(L):
    rows = L // GRID_W
    row = jnp.repeat(jnp.arange(rows), GRID_W).astype(jnp.float32)
    col = jnp.tile(jnp.arange(GRID_W), rows).astype(jnp.float32)
    inv = ROPE_THETA ** (-jnp.arange(ROPE_FREQS, dtype=jnp.float32) / ROPE_FREQS)
    ar = row[:, None] * inv[None, :]
    ac = col[:, None] * inv[None, :]
    ang = jnp.concatenate([ar, ar, ac, ac], axis=-1)
    return jnp.cos(ang), jnp.sin(ang)


def apply_rope(x, cos, sin):
    f = ROPE_FREQS
    xf = x.astype(jnp.float32)
    x1, x2, x3, x4 = xf[..., :f], xf[..., f:2 * f], xf[..., 2 * f:3 * f], xf[..., 3 * f:]
    rot = jnp.concatenate([-x2, x1, -x4, x3], axis=-1)
    return (xf * cos[None, :, None, :] + rot * sin[None, :, None, :]).astype(x.dtype)


def short_conv(p, w):
    b_gate, c_gate, xin = jnp.split(p, 3, axis=-1)
    z = c_gate * xin
    zp = jnp.pad(z, ((0, 0), (1, 1), (0, 0)))
    y = zp[:, :-2] * w[0] + zp[:, 1:-1] * w[1] + zp[:, 2:] * w[2]
    return b_gate * y


def spatial_gating(p, g_norm, w_s, b_s):
    B_, S, _ = p.shape
    z = jax.nn.gelu(p)
    u, v = jnp.split(z, 2, axis=-1)
    v = layernorm(v, g_norm)
    v = v.reshape(B_, S // SGU_CHUNK, SGU_CHUNK, SGU_HEADS, SGU_HEAD_DIM)
    s = jnp.einsum('hpq,bnqhc->bnphc', w_s, v) + b_s.T[None, None, :, :, None]
    return u * s.reshape(B_, S, D_SGU)


def windowed_attention(q, k, v, kc, vc, sink):
    B_, L = q.shape[:2]
    nb = L // BLOCK
    scale = HEAD_DIM ** -0.5
    qb = q.reshape(B_, nb, BLOCK, N_KV_HEADS, Q_PER_KV, HEAD_DIM)
    pad = ((0, 0), (BLOCK, BLOCK), (0, 0), (0, 0))
    kp = jnp.pad(k, pad).reshape(B_, nb + 2, BLOCK, N_KV_HEADS, HEAD_DIM)
    vp = jnp.pad(v, pad).reshape(B_, nb + 2, BLOCK, N_KV_HEADS, HEAD_DIM)
    kb = jnp.concatenate([kp[:, :-2], kp[:, 1:-1], kp[:, 2:]], axis=2)
    vb = jnp.concatenate([vp[:, :-2], vp[:, 1:-1], vp[:, 2:]], axis=2)
    s_loc = jnp.einsum('bnqhgd,bnkhd->bnhgqk', qb, kb).astype(jnp.float32) * scale
    s_ctx = jnp.einsum('bnqhgd,bkhd->bnhgqk', qb, kc).astype(jnp.float32) * scale
    qi = jnp.arange(BLOCK)[:, None]
    ki = jnp.arange(3 * BLOCK)[None, :]
    band = jnp.abs(ki - BLOCK - qi) <= WINDOW
    kblock = jnp.arange(nb)[:, None] + ki // BLOCK - 1
    in_range = (kblock >= 0) & (kblock < nb)
    mask = band[None] & in_range[:, None, :]
    s_loc = jnp.where(mask[None, :, None, None], s_loc, -jnp.inf)
    sk = sink.astype(jnp.float32).reshape(N_KV_HEADS, Q_PER_KV)[None, None, :, :, None]
    m = jnp.maximum(jnp.maximum(jnp.max(s_loc, axis=-1), jnp.max(s_ctx, axis=-1)), sk)
    p_loc = jnp.exp(s_loc - m[..., None])
    p_ctx = jnp.exp(s_ctx - m[..., None])
    denom = jnp.sum(p_loc, axis=-1) + jnp.sum(p_ctx, axis=-1) + jnp.exp(sk - m)
    o = (jnp.einsum('bnhgqk,bnkhd->bnhgqd', p_loc, vb.astype(jnp.float32))
         + jnp.einsum('bnhgqk,bkhd->bnhgqd', p_ctx, vc.astype(jnp.float32))) / denom[..., None]
    return o.transpose(0, 1, 4, 2, 3, 5).reshape(B_, L, D_ATTN).astype(q.dtype)


def context_attention(qc, kc, vc, sink):
    B_, C = qc.shape[:2]
    scale = HEAD_DIM ** -0.5
    qg = qc.reshape(B_, C, N_KV_HEADS, Q_PER_KV, HEAD_DIM)
    s = jnp.einsum('bqhgd,bkhd->bhgqk', qg, kc).astype(jnp.float32) * scale
    sk = sink.astype(jnp.float32).reshape(N_KV_HEADS, Q_PER_KV)[None, :, :, None]
    m = jnp.maximum(jnp.max(s, axis=-1), sk)
    p = jnp.exp(s - m[..., None])
    denom = jnp.sum(p, axis=-1) + jnp.exp(sk - m)
    o = jnp.einsum('bhgqk,bkhd->bhgqd', p, vc.astype(jnp.float32)) / denom[..., None]
    return o.transpose(0, 3, 1, 2, 4).reshape(B_, C, D_ATTN).astype(qc.dtype)


def merge_groups(y_conv, y_sgu, y_attn, g, w_out):
    y = jnp.concatenate([rms_plain(y_conv), rms_plain(y_sgu), rms_plain(y_attn)], axis=-1)
    return (y * g) @ w_out


def token_mixers(hl, hc, w_in, conv_w, sgu_norm_g, sgu_w, sgu_b, sink, mix_norm_g, w_out,
                 cos, sin, need_ctx):
    B_, L, _ = hl.shape
    C = hc.shape[1]
    pl = hl @ w_in
    conv_l = short_conv(pl[..., :CONV_END], conv_w)
    sgu_l = spatial_gating(pl[..., CONV_END:SGU_END], sgu_norm_g, sgu_w, sgu_b)
    q = apply_rope(pl[..., SGU_END:Q_END].reshape(B_, L, N_HEADS, HEAD_DIM), cos, sin)
    k = apply_rope(pl[..., Q_END:K_END].reshape(B_, L, N_KV_HEADS, HEAD_DIM), cos, sin)
    v = pl[..., K_END:].reshape(B_, L, N_KV_HEADS, HEAD_DIM)
    if need_ctx:
        pc = hc @ w_in
        pkv = pc[..., Q_END:]
    else:
        pkv = hc @ w_in[:, Q_END:]
    kc = pkv[..., :N_KV_HEADS * HEAD_DIM].reshape(B_, C, N_KV_HEADS, HEAD_DIM)
    vc = pkv[..., N_KV_HEADS * HEAD_DIM:].reshape(B_, C, N_KV_HEADS, HEAD_DIM)
    attn_l = windowed_attention(q, k, v, kc, vc, sink)
    yl = merge_groups(conv_l, sgu_l, attn_l, mix_norm_g, w_out)
    if not need_ctx:
        return yl, None
    conv_c = short_conv(pc[..., :CONV_END], conv_w)
    sgu_c = spatial_gating(pc[..., CONV_END:SGU_END], sgu_norm_g, sgu_w, sgu_b)
    qc = pc[..., SGU_END:Q_END].reshape(B_, C, N_HEADS, HEAD_DIM)
    attn_c = context_attention(qc, kc, vc, sink)
    yc = merge_groups(conv_c, sgu_c, attn_c, mix_norm_g, w_out)
    return yl, yc


def peer(h, wq, keys, u_tab, v_tab):
    B_, S, D = h.shape
    T = B_ * S
    t = h.reshape(T, D)
    q = (t @ wq).reshape(T, PEER_HEADS, 2, PEER_DHALF)
    s = jnp.einsum('thpd,hpkd->thpk', q, keys).astype(jnp.float32)
    sv, si = lax.top_k(s, PEER_TOPK)
    cand = (sv[..., 0, :, None] + sv[..., 1, None, :]).reshape(T, PEER_HEADS, PEER_TOPK * PEER_TOPK)
    cidx = (si[..., 0, :, None] * N_KEYS + si[..., 1, None, :]).reshape(T, PEER_HEADS, PEER_TOPK * PEER_TOPK)
    fv, fi = lax.top_k(cand, PEER_TOPK)
    experts = jnp.take_along_axis(cidx, fi, axis=-1)
    gates = jax.nn.softmax(fv, axis=-1).astype(h.dtype)
    nc = T // EXPERT_CHUNK
    experts = experts.reshape(nc, EXPERT_CHUNK, PEER_HEADS * PEER_TOPK)
    gates = gates.reshape(nc, EXPERT_CHUNK, PEER_HEADS * PEER_TOPK)
    tc = t.reshape(nc, EXPERT_CHUNK, D)

    def run(args):
        xc, ec, gc = args
        a = jax.nn.gelu(jnp.einsum('tkd,td->tk', u_tab[ec], xc))
        return jnp.einsum('tk,tkd->td', a * gc, v_tab[ec])

    out = lax.map(run, (tc, experts, gates))
    return out.reshape(B_, S, D)


def setup_inputs(seed: int = 0) -> dict:
    key = jax.random.key(seed)
    ks = jax.random.split(key, 24)
    n = jax.random.normal
    f32 = jnp.float32
    return {
        "x": n(ks[0], (BATCH, SEQ, D_MODEL), f32),
        "c": n(ks[1], (BATCH, D_MODEL), f32),
        "ctx": n(ks[2], (BATCH, CTX_LEN, D_MODEL), f32),
        "c_ctx": n(ks[3], (D_MODEL,), f32),
        "w_ada": n(ks[4], (DEPTH, D_MODEL, 6 * D_MODEL), f32) * (0.5 * D_MODEL ** -0.5),
        "b_ada": n(ks[5], (DEPTH, 6 * D_MODEL), f32) * 0.02,
        "norm1_g": 1.0 + 0.05 * n(ks[6], (DEPTH, D_MODEL), f32),
        "norm2_g": 1.0 + 0.05 * n(ks[7], (DEPTH, D_MODEL), f32),
        "w_in": n(ks[8], (DEPTH, D_MODEL, D_IN), f32) * D_MODEL ** -0.5,
        "conv_w": n(ks[9], (DEPTH, CONV_W, D_CONV), f32) * CONV_W ** -0.5,
        "sgu_norm_g": 1.0 + 0.05 * n(ks[10], (DEPTH, D_SGU), f32),
        "sgu_w": n(ks[11], (DEPTH, SGU_HEADS, SGU_CHUNK, SGU_CHUNK), f32) * SGU_CHUNK ** -0.5,
        "sgu_b": 1.0 + 0.1 * n(ks[12], (DEPTH, SGU_HEADS, SGU_CHUNK), f32),
        "attn_sink": 0.5 * n(ks[13], (DEPTH, N_HEADS), f32),
        "mix_norm_g": 1.0 + 0.05 * n(ks[14], (DEPTH, D_MIX), f32),
        "w_out": n(ks[15], (DEPTH, D_MIX, D_MODEL), f32) * D_MIX ** -0.5,
        "peer_wq": n(ks[16], (DEPTH, D_MODEL, PEER_HEADS * PEER_DQ), f32) * D_MODEL ** -0.5,
        "peer_keys": n(ks[17], (DEPTH, PEER_HEADS, 2, N_KEYS, PEER_DHALF), f32) * PEER_DHALF ** -0.5,
        "peer_u": n(ks[18], (DEPTH, N_EXPERTS, D_MODEL), f32) * D_MODEL ** -0.5,
        "peer_v": n(ks[19], (DEPTH, N_EXPERTS, D_MODEL), f32) * 0.5,
        "final_g": 1.0 + 0.05 * n(ks[20], (D_MODEL,), f32),
    }


def reference(x, c, ctx, c_ctx, w_ada, b_ada, norm1_g, norm2_g, w_in, conv_w, sgu_norm_g, sgu_w,
              sgu_b, attn_sink, mix_norm_g, w_out, peer_wq, peer_keys, peer_u, peer_v, final_g):
    L = x.shape[1]
    C = ctx.shape[1]
    cos, sin = axial_rope_tables(L)
    sc = jax.nn.silu(c)
    scc = jax.nn.silu(c_ctx)
    xl, xc = x, ctx
    for i in range(DEPTH):
        last = i == DEPTH - 1
        mod_l = (sc @ w_ada[i] + b_ada[i])[:, None, :]
        mod_c = scc @ w_ada[i] + b_ada[i]
        sh1, s1, g1, sh2, s2, g2 = jnp.split(mod_l, 6, axis=-1)
        csh1, cs1, cg1, csh2, cs2, cg2 = jnp.split(mod_c, 6, axis=-1)
        hl = rmsnorm(xl, norm1_g[i]) * (1.0 + s1) + sh1
        hc = rmsnorm(xc, norm1_g[i]) * (1.0 + cs1) + csh1
        yl, yc = token_mixers(hl, hc, w_in[i], conv_w[i], sgu_norm_g[i], sgu_w[i], sgu_b[i],
                              attn_sink[i], mix_norm_g[i], w_out[i], cos, sin, not last)
        xl = xl + g1 * yl
        hl = rmsnorm(xl, norm2_g[i]) * (1.0 + s2) + sh2
        if last:
            xl = xl + g2 * peer(hl, peer_wq[i], peer_keys[i], peer_u[i], peer_v[i])
        else:
            xc = xc + cg1 * yc
            hc = rmsnorm(xc, norm2_g[i]) * (1.0 + cs2) + csh2
            f = peer(jnp.concatenate([hc, hl], axis=1), peer_wq[i], peer_keys[i], peer_u[i], peer_v[i])
            xc = xc + cg2 * f[:, :C]
            xl = xl + g2 * f[:, C:]
    return rmsnorm(xl, final_g)
```

```python
import numpy as np
from contextlib import ExitStack
import concourse.bass as bass
import concourse.mybir as mybir
from concourse.bass_utils import run_bass_kernel_spmd

F32 = mybir.dt.float32
BF16 = mybir.dt.bfloat16
U32 = mybir.dt.uint32
AF = mybir.ActivationFunctionType
ALU = mybir.AluOpType
AX = mybir.AxisListType

D = 1024
EPS = 1e-6
NCOLD = 2688
C_BG, C_CG, C_XI, C_SGU, C_V, C_Q, C_K, C_QP, C_KP = 0, 256, 512, 768, 1280, 1408, 1920, 2048, 2560
EPOCH = 20000


def KN(t):
    return t.name.split("__")[0]


class Sched:
    ENG = ("pe", "dve", "act", "pool", "sp")

    def __init__(self, nc, es, dma_slots, needed=None):
        self.nc = nc
        self.es = es
        self.eng = {"pe": nc.tensor, "dve": nc.vector, "act": nc.scalar, "pool": nc.gpsimd, "sp": nc.sync}
        self.needed = needed
        self.waited = set()
        self.sem = {}
        self.semname = {}
        self.cnt = {e: 0 for e in self.ENG}
        self.seq = {e: 0 for e in self.ENG}
        self.epoch = {e: -1 for e in self.ENG}
        self.semval = {}
        self.known = {e: {} for e in self.ENG}
        self.lastw = {}
        self.readers = {}
        self.ninst = {e: 0 for e in self.ENG}
        self.ninc = 0
        for e in self.ENG:
            self._new_epoch(e)
        self.dsem = {q: [es.enter_context(nc.semaphore(f"d_{q}{i}")) for i in range(n)] for q, n in dma_slots.items()}
        self.dcnt = {q: [0] * n for q, n in dma_slots.items()}
        self.dnext = {q: 0 for q in dma_slots}

    def _new_epoch(self, e):
        self.epoch[e] += 1
        name = f"s_{e}{self.epoch[e]}"
        self.sem[e] = self.es.enter_context(self.nc.semaphore(name))
        self.semname[e] = name
        self.cnt[e] = 0

    def _wait(self, e, prod):
        kind, pid, val = prod[0], prod[1], prod[2]
        k = self.known[e]
        if k.get((kind, pid), 0) >= val:
            return
        k[(kind, pid)] = val
        if kind == "c":
            self.waited.add((pid, val))
            semh, sval = self.semval[(pid, val)] if self.needed is not None else self.semval_all[(pid, val)]
            self.eng[e].wait_ge(semh, sval)
        else:
            self.eng[e].wait_ge(prod[4], val)
        self.ninst[e] += 1

    semval_all = None

    def _deps(self, e, reads, writes, is_dma):
        deps = []
        for k in reads:
            w = self.lastw.get(k)
            if w is not None:
                if is_dma or not (w[3] == e and e == "pe"):
                    deps.append(w)
        for k in writes:
            w = self.lastw.get(k)
            if w is not None and (is_dma or w[3] != e):
                deps.append(w)
            for r in self.readers.get(k, {}).values():
                if is_dma or r[3] != e:
                    deps.append(r)
        for d in deps:
            self._wait(e, d)

    def _record(self, prod, reads, writes):
        for k in writes:
            self.lastw[k] = prod
            self.readers[k] = {}
        for k in reads:
            self.readers.setdefault(k, {})[(prod[0], prod[1])] = prod

    def op(self, e, fn, reads, writes):
        reads = [r for r in reads if r is not None]
        self._deps(e, reads, writes, False)
        self.seq[e] += 1
        me = (e, self.seq[e])
        inst = fn(self.eng[e])
        self.ninst[e] += 1
        if self.needed is None or me in self.needed:
            if self.cnt[e] >= EPOCH:
                self._new_epoch(e)
            self.cnt[e] += 1
            inst.then_inc(self.sem[e], 1)
            self.ninc += 1
            if self.needed is None:
                if self.semval_all is None:
                    self.semval_all = {}
                self.semval_all[me] = (self.sem[e], self.cnt[e])
            else:
                self.semval[me] = (self.sem[e], self.cnt[e])
        prod = ("c", e, self.seq[e], e)
        self._record(prod, reads, writes)
        return prod

    def dma(self, q, out, in_, reads=None, writes=None, slow=False):
        reads = [KN(in_)] if reads is None else reads
        writes = [KN(out)] if writes is None else writes
        slot = self.dnext[q]
        self.dnext[q] = (slot + 1) % len(self.dsem[q])
        semh = self.dsem[q][slot]
        name = f"d_{q}{slot}"
        if self.dcnt[q][slot] > 0:
            self._wait(q, ("d", name, self.dcnt[q][slot] * 16, "dma", semh))
        self._deps(q, reads, writes, True)
        self.dcnt[q][slot] += 1
        kw = {"allow_slow_non_contiguous": True} if slow else {}
        self.eng[q].dma_start(out=out, in_=in_, **kw).then_inc(semh, 16)
        self.ninst[q] += 1
        prod = ("d", name, self.dcnt[q][slot] * 16, "dma", semh)
        self._record(prod, reads, writes)
        return prod

    def barrier(self):
        prods = []
        for e in self.ENG:
            if self.seq[e] > 0:
                prods.append(("c", e, self.seq[e], e))
        for q in self.dsem:
            for i, s in enumerate(self.dsem[q]):
                if self.dcnt[q][i] > 0:
                    prods.append(("d", f"d_{q}{i}", self.dcnt[q][i] * 16, "dma", s))
        for e in self.ENG:
            for p in prods:
                if p[3] != e:
                    self._wait(e, p)
        self.lastw.clear()
        self.readers.clear()

    @staticmethod
    def _k(*aps):
        return [KN(a) for a in aps if hasattr(a, "name")]

    def mm(self, out, lhsT, rhs, start=True, stop=True, rk=None, wk=None):
        return self.op("pe", lambda e: e.matmul(out, lhsT=lhsT, rhs=rhs, start=start, stop=stop),
                       self._k(lhsT, rhs) if rk is None else rk, self._k(out) if wk is None else wk)

    def tr(self, out, in_, ident, rk=None, wk=None):
        return self.op("pe", lambda e: e.transpose(out=out, in_=in_, identity=ident),
                       self._k(in_, ident) if rk is None else rk, self._k(out) if wk is None else wk)

    def act(self, out, in_, func, bias=None, scale=None, accum_out=None, rk=None, wk=None):
        kw = {}
        if bias is not None:
            kw["bias"] = bias
        if scale is not None:
            kw["scale"] = scale
        if accum_out is not None:
            kw["accum_out"] = accum_out
        r = self._k(in_, bias, scale) if rk is None else rk
        w = self._k(out, accum_out) if wk is None else wk
        return self.op("act", lambda e: e.activation(out=out, in_=in_, func=func, **kw), r, w)

    def tt(self, e, out, in0, in1, op, rk=None, wk=None):
        return self.op(e, lambda g: g.tensor_tensor(out=out, in0=in0, in1=in1, op=op),
                       self._k(in0, in1) if rk is None else rk, self._k(out) if wk is None else wk)

    def ts(self, out, in0, s1, s2=None, op0=ALU.mult, op1=None, rk=None, wk=None):
        kw = {} if op1 is None else {"op1": op1}
        return self.op("dve", lambda g: g.tensor_scalar(out=out, in0=in0, scalar1=s1, scalar2=s2, op0=op0, **kw),
                       self._k(in0, s1, s2) if rk is None else rk, self._k(out) if wk is None else wk)

    def stt(self, out, in0, scalar, in1, op0, op1, rk=None, wk=None):
        return self.op("dve", lambda g: g.scalar_tensor_tensor(out=out, in0=in0, scalar=scalar, in1=in1, op0=op0, op1=op1),
                       self._k(in0, scalar, in1) if rk is None else rk, self._k(out) if wk is None else wk)

    def cp(self, e, out, in_, rk=None, wk=None):
        r = self._k(in_) if rk is None else rk
        w = self._k(out) if wk is None else wk
        if e == "act":
            return self.op("act", lambda g: g.activation(out=out, in_=in_, func=AF.Copy), r, w)
        return self.op(e, lambda g: g.tensor_copy(out=out, in_=in_), r, w)

    def dv(self, name, reads, writes, **kw):
        return self.op("dve", lambda g: getattr(g, name)(**kw), reads, writes)


def build_program(NB, DEPTH=2):
    needed = _build(NB, DEPTH, None)[1]
    return _build(NB, DEPTH, needed)[0]


def _build(NB, DEPTH, needed):
    NKV = NB + 4
    NLAT = NKV * 128
    NTOK = NLAT + 256
    NBLK = NKV + 2
    ZW = NTOK + 4
    nc = bass.Bass("TRN2", target_bir_lowering=False)

    def din(name, shape, dt=F32):
        return nc.dram_tensor(name, list(shape), dt, kind="ExternalInput").ap()

    def dscr(name, shape, dt=F32):
        return nc.dram_tensor(name, list(shape), dt, kind="Internal").ap()

    x_ext = din("x_ext", [NLAT, D])
    ctx_in = din("ctx_in", [256, D])
    cT_in = din("cT", [128, 16])
    validT_in = din("validT", [128, NBLK])
    cos_in = din("cosT", [128, NTOK])
    sin_in = din("sinT", [128, NTOK])
    ident_in = din("ident", [128, 128])
    iota_in = din("iota128", [128, 128])
    iota16_in = din("iota16", [128, 16])
    masks_in = din("masks", [128, 256])
    fg_in = din("final_g", [1, D])
    L = []
    for l in range(DEPTH):
        L.append(dict(
            wada=din(f"wada{l}", [D, 6 * D]), bada=din(f"bada{l}", [1, 6 * D]),
            n1g=din(f"n1g{l}", [1, D]), n2g=din(f"n2g{l}", [1, D]),
            wd=din(f"wd{l}", [D, NCOLD]), cw=din(f"cw{l}", [128, 6]),
            sgg=din(f"sgg{l}", [1, 256]), wsT=din(f"wsT{l}", [128, 512]), sbT=din(f"sbT{l}", [128, 4]),
            sink=din(f"sink{l}", [1, 8]), gmix=din(f"gmix{l}", [1, D]), gmT=din(f"gmT{l}", [128, 8]),
            wo=din(f"wo{l}", [D, D]), wq=din(f"wq{l}", [D, 2048]), keysT=din(f"keysT{l}", [128, 2048]),
            ut=din(f"ut{l}", [D, 16384]), vp=din(f"vp{l}", [16384, D]),
            utb=dscr(f"utb{l}", [D, 16384], BF16), vb=dscr(f"vb{l}", [16384, D], BF16),
        ))
    out_d = nc.dram_tensor("out", [NB * 128, D], F32, kind="ExternalOutput").ap()
    xmid = [dscr(f"xmid{l}", [NBLK * 128, D]) for l in range(DEPTH)]
    x1 = dscr("x1", [NBLK * 128, D])

    es = ExitStack()
    with es:
        K = Sched(nc, es, {"sp": 16, "act": 4, "pool": 20}, needed)

        uid = [0]

        def sb(name, shape, dt=F32, st=None):
            uid[0] += 1
            return (st or es).enter_context(nc.sbuf_tensor(f"{name}__{uid[0]}", list(shape), dt))

        R = [es.enter_context(nc.psum_tensor(f"R{i}", [128, 512], F32)) for i in range(8)]
        ident = sb("ident", [128, 128])
        iota128 = sb("iota128", [128, 128])
        iota16 = sb("iota16", [128, 16])
        masks = sb("masks", [128, 256], BF16)
        validT = sb("validT", [128, NBLK])
        cT = sb("cT", [128, 16])
        scT = sb("scT", [128, 16])
        ones_bf = sb("ones_bf", [128, 128], BF16)
        ones_f = sb("ones_f", [1, 128])
        fg_bc = sb("fg_bc", [128, D])
        stat = sb("stat", [128, 64])
        junk = sb("junk", [128, D], BF16)

        K.dma("sp", ident[:], ident_in[:, :])
        K.dma("sp", iota128[:], iota_in[:, :])
        K.dma("sp", iota16[:], iota16_in[:, :])
        K.dma("pool", masks[:], masks_in[:, :])
        K.dma("sp", validT[:], validT_in[:, :])
        K.dma("sp", cT[:], cT_in[:, :])
        K.dma("sp", fg_bc[:], fg_in.partition_broadcast(128))
        K.op("dve", lambda g: g.memset(ones_bf[:], 1.0), [], ["ones_bf"])
        K.op("dve", lambda g: g.memset(ones_f[:], 1.0), [], ["ones_f"])
        K.act(scT[:], cT[:], AF.Silu)
        thr16 = sb("thr16", [128, 16])
        K.ts(thr16[:], iota16[:], 16.0, None, ALU.mult)

        conv_jobs = []
        for l in range(DEPTH):
            jobs = []
            for r in range(8):
                for cc in range(4):
                    jobs.append((L[l]["utb"][r * 128:(r + 1) * 128, cc * 4096:(cc + 1) * 4096],
                                 L[l]["ut"][r * 128:(r + 1) * 128, cc * 4096:(cc + 1) * 4096]))
            vsrc = L[l]["vp"].rearrange("(a p) d -> p a d", p=128)
            vdst = L[l]["vb"].rearrange("(a p) d -> p a d", p=128)
            for a0 in range(0, 128, 8):
                jobs.append((vdst[:, a0:a0 + 8, :], vsrc[:, a0:a0 + 8, :]))
            conv_jobs.append(jobs)

        def conv_pump(l, n):
            for _ in range(n):
                if conv_jobs[l]:
                    o, i = conv_jobs[l].pop(0)
                    K.dma("pool", o, i, reads=[], writes=[f"cv{l}"])

        def rstd_from_ss(dst, ss, n):
            K.act(dst, ss, AF.Ln, bias=EPS, scale=1.0 / n)
            K.act(dst, dst, AF.Exp, scale=-0.5)

        def emit_mod(l, specs):
            with ExitStack() as st:
                wa = [sb(f"wa{i}", [128, 8, 256], st=st) for i in range(2)]
                ba = [sb(f"ba{i}", [1, 256], st=st) for i in range(2)]
                screp = [sb(f"screp{i}", [128, 8, 128], st=st) for i in range(2)]
                gb = sb("gb_bc", [128, D], st=st)
                scv = scT[:].rearrange("p (c r) -> p c r", r=2)
                for s_ in range(2):
                    K.cp("dve", screp[s_][:], scv[:, :, s_:s_ + 1].to_broadcast([128, 8, 128]))
                wsrc = L[l]["wada"].rearrange("(c p) n -> p c n", p=128)
                n = 0
                for (chunk, dsts, gd) in specs:
                    if gd is not None:
                        K.dma("sp", gb[:], gd.partition_broadcast(128))
                    for q in range(4):
                        col = chunk * D + q * 256
                        w = wa[n % 2]
                        b_ = ba[n % 2]
                        K.dma("sp", w[:], wsrc[:, :, col:col + 256])
                        K.dma("sp", b_[:], L[l]["bada"][:, col:col + 256])
                        for s_ in range(2):
                            ps = R[(2 * n + s_) % 4]
                            for c in range(8):
                                K.mm(ps[:, 0:256], screp[s_][:, c, :], w[:, c, :], start=(c == 0), stop=False)
                            K.mm(ps[:, 0:256], ones_f[:, :], b_[:, :], start=False, stop=True)
                            d_ = dsts[s_][:, q * 256:(q + 1) * 256]
                            if gd is None:
                                K.cp("act", d_, ps[:, 0:256])
                            else:
                                K.stt(d_, ps[:, 0:256], 1.0, gb[:, q * 256:(q + 1) * 256], ALU.add, ALU.mult)
                        n += 1
                K.barrier()

        def norm_mod(xt, hf, gm, sh, vcol, sc):
            ss, rs = stat[:, sc:sc + 1], stat[:, sc + 1:sc + 2]
            K.act(junk[:], xt, AF.Square, accum_out=ss)
            rstd_from_ss(rs, ss, D)
            if vcol is not None:
                K.tt("dve", rs, rs, vcol, ALU.mult)
                K.stt(hf, xt, rs, gm, ALU.mult, ALU.mult)
                K.stt(hf, sh, vcol, hf, ALU.mult, ALU.add)
            else:
                K.stt(hf, xt, rs, gm, ALU.mult, ALU.mult)
                K.tt("dve", hf, hf, sh, ALU.add)

        def transpose_to(hf, nch, dst_fn, banks=(0, 1)):
            for c0 in range(0, nch, 4):
                n = min(4, nch - c0)
                ps = R[banks[(c0 // 4) % 2]]
                for k in range(n):
                    K.tr(ps[:, k * 128:(k + 1) * 128], hf[:, (c0 + k) * 128:(c0 + k + 1) * 128], ident[:])
                K.cp("act", dst_fn(c0, n), ps[:, 0:n * 128].rearrange("p (c t) -> p c t", c=n))

        zD = dscr("zD", [128, 2, ZW], BF16)
        bgD = dscr("bgD", [128, 2, ZW], BF16)
        for l in range(DEPTH):
            P = L[l]
            last = (l == DEPTH - 1)
            lat_lo = 0 if l == 0 else 1
            lat_hi = NKV - lat_lo
            post_lo, post_hi = lat_lo + 1, lat_hi - 1
            xsrc_lat = x_ext if l == 0 else x1
            xdst = xmid[l]

            def xrows(b, l=l, xsrc_lat=xsrc_lat):
                if b >= NKV:
                    return (ctx_in[(b - NKV) * 128:(b - NKV + 1) * 128, :] if l == 0 else x1[b * 128:(b + 1) * 128, :])
                return xsrc_lat[b * 128:(b + 1) * 128, :]

            with ExitStack() as st:
                gm1 = [sb(f"gm1_{s}", [128, D], F32, st) for s in range(2)]
                sh1 = [sb(f"sh1_{s}", [128, D], F32, st) for s in range(2)]
                g1 = [sb(f"g1_{s}", [128, D], F32, st) for s in range(2)]
                emit_mod(l, [(0, sh1, None), (1, gm1, P["n1g"]), (2, g1, None)])
                wd = sb("wd", [128, 8, NCOLD], BF16, st)
                wo = sb("wo", [128, 8, D], BF16, st)
                cosg = sb("cosg", [128, 512], BF16, st)
                sing = sb("sing", [128, 512], BF16, st)
                KT = sb("KT", [128, NTOK], BF16, st)
                Va = sb("Va", [128, NBLK, 2, 65], BF16, st)
                zg = sb("zg", [128, 2, 512], BF16, st)
                bgg = sb("bgg", [128, 2, 512], BF16, st)
                zb = sb("zb", [128, 2, 130], BF16, st)
                bgb = sb("bgb", [128, 2, 128], BF16, st)
                zpad = sb("zpad", [128, 2, 1], BF16, st)
                QT = [sb(f"QT{i}", [128, 4, 512], BF16, st) for i in range(2)]
                ysgT = [sb(f"ysgT{i}", [128, 2, 512], BF16, st) for i in range(2)]
                hTg = sb("hTg", [128, 8, 512], BF16, st)
                xt = [sb(f"xt{i}", [128, D], F32, st) for i in range(2)]
                hf = sb("hf", [128, D], F32, st)
                gmix_bc = sb("gmix_bc", [128, D], F32, st)
                gmT = sb("gmT", [128, 8], F32, st)
                cw = sb("cw", [128, 6], F32, st)
                sgg_bc = sb("sgg_bc", [128, 256], F32, st)
                wsT = sb("wsT", [128, 512], BF16, st)
                sbT = sb("sbT", [128, 4], F32, st)
                esink = sb("esink", [128, 8], F32, st)
                tmpA = sb("tmpA", [128, 512], F32, st)
                tmpB = sb("tmpB", [128, 512], F32, st)
                cgt = sb("cgt", [128, 512], F32, st)
                zs = sb("zs", [128, 512], F32, st)
                vln = sb("vln", [128, 256], F32, st)
                vlb = sb("vlb", [128, 256], BF16, st)
                ysg = sb("ysg", [128, 256], F32, st)
                bnst = sb("bnst", [128, 8], F32, st)
                PT = sb("PT", [128, 5, 512], BF16, st)
                att = sb("att", [128, 512], F32, st)
                yc = sb("yc", [128, 2, 128], F32, st)
                ycq = sb("ycq", [128, 2, 128], BF16, st)
                ca = sb("ca", [128, 128], F32, st)
                rsb = sb("rsb", [128, 128], F32, st)
                ynT = sb("ynT", [128, 8, 128], BF16, st)
                xr = sb("xr", [128, D], F32, st)
                xn = sb("xn", [128, D], F32, st)

                wdsrc = P["wd"].rearrange("(c p) n -> p c n", p=128)
                wosrc = P["wo"].rearrange("(c p) n -> p c n", p=128)
                for c in range(8):
                    K.dma("pool", wd[:, c, :], wdsrc[:, c, :], writes=[f"wd{c}"])
                    K.dma("pool", wo[:, c, :], wosrc[:, c, :], writes=[f"wo{c}"])
                K.dma("pool", wsT[:], P["wsT"][:, :])
                K.dma("sp", gmix_bc[:], P["gmix"].partition_broadcast(128))
                K.dma("sp", sgg_bc[:], P["sgg"].partition_broadcast(128))
                K.dma("sp", esink[:], P["sink"].partition_broadcast(128))
                K.dma("sp", gmT[:], P["gmT"][:, :])
                K.dma("sp", cw[:], P["cw"][:, :])
                K.dma("sp", sbT[:], P["sbT"][:, :])
                K.act(esink[:], esink[:], AF.Exp)
                K.op("dve", lambda g: g.memset(zpad[:], 0.0), [], ["zpad"])
                for pc in (0, NLAT + 1, NLAT + 2, NTOK + 3):
                    K.dma("sp", zD[:, :, pc:pc + 1], zpad[:], writes=[f"zp{pc}"], slow=True)
                WDK = [f"wd{c}" for c in range(8)]
                WOK = [f"wo{c}" for c in range(8)]
                ZPK = [f"zp{pc}" for pc in (0, NLAT + 1, NLAT + 2, NTOK + 3)]

                def zoff(b):
                    return 1 + b * 128 + (2 if b >= NKV else 0)

                def m1_group(gi, blks):
                    s = 1 if blks[0] >= NKV else 0
                    par = gi % 2
                    N = 128 * len(blks)
                    tok0 = blks[0] * 128
                    z0 = zoff(blks[0])
                    K.dma("pool", cosg[:, 0:N], cos_in[:, tok0:tok0 + N])
                    K.dma("pool", sing[:, 0:N], sin_in[:, tok0:tok0 + N])
                    for bi, b in enumerate(blks):
                        x_ = xt[bi % 2]
                        K.dma("sp", x_[:], xrows(b))
                        norm_mod(x_[:], hf[:], gm1[s][:], sh1[s][:], validT[:, b:b + 1], 0)
                        transpose_to(hf, 8, lambda c0, n, bi=bi: hTg[:, c0:c0 + n, bi * 128:(bi + 1) * 128])
                        yield

                    def proj(col0, bank):
                        ps = R[bank]
                        for c in range(8):
                            K.mm(ps[:, 0:N], wd[:, c, col0:col0 + 128], hTg[:, c, 0:N], start=(c == 0), stop=(c == 7),
                                 rk=[WDK[c], "hTg"])
                        return ps
                    for j in range(2):
                        ps = proj(C_BG + j * 128, 2)
                        K.cp("act", bgg[:, j, 0:N], ps[:, 0:N])
                        ps = proj(C_CG + j * 128, 3)
                        K.cp("act", cgt[:, 0:N], ps[:, 0:N])
                        ps = proj(C_XI + j * 128, 2)
                        K.tt("dve", zg[:, j, 0:N], ps[:, 0:N], cgt[:, 0:N], ALU.mult)
                        yield
                    K.dma("sp", zD[:, :, z0:z0 + N], zg[:, :, 0:N], writes=[f"z{gi}"])
                    K.dma("sp", bgD[:, :, z0:z0 + N], bgg[:, :, 0:N], writes=[f"bg{gi}"])

                    def rope(ca_, cb_, dst, wk):
                        pa = proj(ca_, 2)
                        pb = proj(cb_, 3)
                        K.tt("dve", tmpA[:, 0:N], pa[:, 0:N], cosg[:, 0:N], ALU.mult)
                        K.tt("dve", tmpB[:, 0:N], pb[:, 0:N], sing[:, 0:N], ALU.mult)
                        K.tt("pool", dst, tmpA[:, 0:N], tmpB[:, 0:N], ALU.add, wk=wk)
                    for c in range(4):
                        rope(C_Q + c * 128, C_QP + c * 128, QT[par][:, c, 0:N], [KN(QT[par])])
                        yield
                    rope(C_K, C_KP, KT[:, tok0:tok0 + N], [f"kt{b_}" for b_ in blks])
                    yield
                    for bi, b in enumerate(blks):
                        tsl = slice(bi * 128, (bi + 1) * 128)
                        ps = R[4]
                        for c in range(8):
                            K.mm(ps[:, :], hTg[:, c, tsl], wd[:, c, C_SGU:C_SGU + 512], start=(c == 0), stop=(c == 7),
                                 rk=[WDK[c], "hTg"])
                        K.act(zs[:], ps[:, :], AF.Gelu_apprx_tanh)
                        yield
                        K.dv("bn_stats", ["zs"], ["bnst"], out=bnst[:, 0:6], in_=zs[:, 256:512])
                        K.dv("bn_aggr", ["bnst"], ["bnst2"], out=bnst[:, 6:8], in_=bnst[:, 0:6])
                        K.act(stat[:, 9:10], bnst[:, 7:8], AF.Ln, bias=EPS, rk=["bnst2"], wk=["st9"])
                        K.act(stat[:, 10:11], stat[:, 9:10], AF.Exp, scale=-0.5, rk=["st9"], wk=["st10"])
                        K.ts(vln[:], zs[:, 256:512], bnst[:, 6:7], stat[:, 10:11], ALU.subtract, ALU.mult,
                             rk=["zs", "bnst2", "st10"])
                        K.tt("dve", vlb[:], vln[:], sgg_bc[:], ALU.mult)
                        p5 = R[5]
                        for hh in range(4):
                            K.mm(p5[:, hh * 64:(hh + 1) * 64], wsT[:, hh * 128:(hh + 1) * 128], vlb[:, hh * 64:(hh + 1) * 64])
                        for hh in range(4):
                            K.stt(ysg[:, hh * 64:(hh + 1) * 64], p5[:, hh * 64:(hh + 1) * 64], sbT[:, hh:hh + 1],
                                  zs[:, hh * 64:(hh + 1) * 64], ALU.add, ALU.mult)
                        K.act(junk[:, 0:256], ysg[:], AF.Square, accum_out=stat[:, 12:13], wk=["junk", "st12"])
                        K.act(stat[:, 13:14], stat[:, 12:13], AF.Ln, bias=EPS, scale=1.0 / 256, rk=["st12"], wk=["st13"])
                        K.act(stat[:, 14:15], stat[:, 13:14], AF.Exp, scale=-0.5, rk=["st13"], wk=["st14"])
                        K.stt(vln[:], ysg[:], stat[:, 14:15], gmix_bc[:, 256:512], ALU.mult, ALU.mult, rk=["ysg", "st14", "gmix_bc"])
                        transpose_to(vln, 2, lambda c0, n, bi=bi: ysgT[par][:, c0:c0 + n, bi * 128:(bi + 1) * 128])
                        yield
                        for c in range(8):
                            K.mm(p5[:, 256:384], hTg[:, c, tsl], wd[:, c, C_V:C_V + 128], start=(c == 0), stop=(c == 7),
                                 rk=[WDK[c], "hTg"])
                        K.cp("act", Va[:, b, :, 0:64], p5[:, 256:384].rearrange("p (g d) -> p g d", g=2), wk=[f"vv{b}"])
                        K.cp("dve", Va[:, b, :, 64:65], validT[:, b:b + 1].unsqueeze(2).to_broadcast([128, 2, 1]),
                             wk=[f"v1{b}"])
                        yield

                def post_block(gi, bi, b):
                    s = 1 if b >= NKV else 0
                    par = gi % 2
                    zo = zoff(b)
                    tsl = slice(bi * 128, (bi + 1) * 128)
                    ZK = [f"z{g_}" for g_ in (gi - 1, gi, gi + 1)] + ZPK
                    K.dma("sp", zb[:], zD[:, :, zo - 1:zo + 129], reads=ZK)
                    K.dma("sp", bgb[:], bgD[:, :, zo:zo + 128], reads=[f"bg{gi}"])
                    K.dma("sp", xr[:], xrows(b))
                    for j in range(2):
                        K.ts(ca[:], zb[:, j, 0:128], cw[:, j * 3:j * 3 + 1], None, ALU.mult)
                        K.stt(ca[:], zb[:, j, 1:129], cw[:, j * 3 + 1:j * 3 + 2], ca[:], ALU.mult, ALU.add)
                        K.stt(ca[:], zb[:, j, 2:130], cw[:, j * 3 + 2:j * 3 + 3], ca[:], ALU.mult, ALU.add)
                        K.tt("dve", yc[:, j, :], ca[:], bgb[:, j, :], ALU.mult)
                    K.tt("pool", ycq[:], yc[:], yc[:], ALU.mult)
                    ps = R[5]
                    for j in range(2):
                        K.mm(ps[:, 384:512], ones_bf[:], ycq[:, j, :], start=(j == 0), stop=(j == 1))
                    K.act(rsb[:], ps[:, 384:512], AF.Ln, bias=EPS, scale=1.0 / 256)
                    K.act(rsb[:], rsb[:], AF.Exp, scale=-0.5)
                    for j in range(2):
                        K.stt(ynT[:, j, :], yc[:, j, :], gmT[:, j:j + 1], rsb[:], ALU.mult, ALU.mult, wk=[f"yn{j}"])
                    yield
                    if b >= NKV:
                        keys = [(NKV, None), (NKV + 1, None)]
                    else:
                        keys = [(b - 1, 0), (b, None), (b + 1, 1), (NKV, None), (NKV + 1, None)]
                    for grp in range(2):
                        rows = slice(grp * 64, grp * 64 + 64)
                        for ki, (kb, m) in enumerate(keys):
                            S = R[2 + ki % 2]
                            K.mm(S[:, :].rearrange("p (c t) -> p c t", c=4), KT[rows, kb * 128:(kb + 1) * 128],
                                 QT[par][rows, :, tsl], rk=[f"kt{kb}", KN(QT[par])])
                            K.act(PT[:, ki, :], S[:, :], AF.Exp, scale=0.125, wk=[f"PT{ki}"])
                            if m is not None:
                                pv = PT[:, ki, :].rearrange("p (c t) -> p c t", c=4)
                                K.tt("pool", pv, pv, masks[:, m * 128:(m + 1) * 128].unsqueeze(1).to_broadcast([128, 4, 128]),
                                     ALU.mult, rk=[f"PT{ki}", "masks"], wk=[f"PT{ki}"])
                        O = R[4]
                        Ov = O[:, :].rearrange("p (c d) -> p c d", c=4)
                        for c in range(4):
                            for ki, (kb, m) in enumerate(keys):
                                K.mm(Ov[:, c, 0:65], PT[:, ki, c * 128:(c + 1) * 128], Va[:, kb, grp, :],
                                     start=(ki == 0), stop=(ki == len(keys) - 1), rk=[f"PT{ki}", f"vv{kb}", f"v1{kb}"])
                        den = stat[:, 16:20]
                        K.tt("dve", den.unsqueeze(2), Ov[:, :, 64:65], esink[:, grp * 4:grp * 4 + 4].unsqueeze(2), ALU.add,
                             wk=["den"])
                        K.dv("reciprocal", ["den"], ["rden"], out=stat[:, 20:24], in_=den)
                        K.tt("dve", att[:, grp * 256:(grp + 1) * 256].rearrange("p (c d) -> p c d", c=4), Ov[:, :, 0:64],
                             stat[:, 20:24].unsqueeze(2).to_broadcast([128, 4, 64]), ALU.mult, rk=[KN(O), "rden"])
                        yield
                    K.act(junk[:, 0:512], att[:], AF.Square, accum_out=stat[:, 24:25], wk=["junk", "st24"])
                    K.act(stat[:, 25:26], stat[:, 24:25], AF.Ln, bias=EPS, scale=1.0 / 512, rk=["st24"], wk=["st25"])
                    K.act(stat[:, 26:27], stat[:, 25:26], AF.Exp, scale=-0.5, rk=["st25"], wk=["st26"])
                    K.stt(att[:], att[:], stat[:, 26:27], gmix_bc[:, 512:1024], ALU.mult, ALU.mult, rk=["att", "st26", "gmix_bc"])
                    transpose_to(att, 4, lambda c0, n: ynT[:, 4 + c0:4 + c0 + n, :])
                    yield
                    for hh in range(2):
                        ps = R[6 + hh]
                        for k in range(8):
                            lhs = ysgT[par][:, k - 2, tsl] if k in (2, 3) else ynT[:, k, :]
                            K.mm(ps[:, :], lhs, wo[:, k, hh * 512:(hh + 1) * 512], start=(k == 0), stop=(k == 7),
                                 rk=[KN(ysgT[par]) if k in (2, 3) else ("ynT" if k >= 4 else f"yn{k}"), WOK[k]])
                        K.tt("dve", xn[:, hh * 512:(hh + 1) * 512], ps[:, :], g1[s][:, hh * 512:(hh + 1) * 512], ALU.mult,
                             wk=[f"xn{hh}"])
                        K.tt("pool", xn[:, hh * 512:(hh + 1) * 512], xn[:, hh * 512:(hh + 1) * 512],
                             xr[:, hh * 512:(hh + 1) * 512], ALU.add, rk=[f"xn{hh}", "xr"], wk=[f"xn{hh}"])
                        yield
                    K.dma("sp", xdst[b * 128:(b + 1) * 128, :], xn[:], reads=["xn0", "xn1"], writes=[f"xm{b}"])

                groups = [[NKV, NKV + 1]]
                bl = list(range(lat_lo, lat_hi))
                for i in range(0, len(bl), 4):
                    groups.append(bl[i:i + 4])
                do_ctx_post = not last

                def post_gens(pg, which):
                    pgi, pbl = pg
                    for bi, b in enumerate(pbl):
                        is_last = (bi == len(pbl) - 1)
                        if which == "early" and is_last:
                            continue
                        if which == "late" and not is_last:
                            continue
                        if b >= NKV:
                            if do_ctx_post:
                                yield from post_block(pgi, bi, b)
                        elif post_lo <= b < post_hi:
                            yield from post_block(pgi, bi, b)
                prev = None
                for gi, blks in enumerate(groups):
                    gm = m1_group(gi, blks)
                    gp = post_gens(prev, "early") if (prev is not None and prev[0] > 0) else None
                    if prev is not None and prev[0] == 0:
                        gp = post_gens(prev, "all")
                    while gm is not None or gp is not None:
                        if gm is not None and next(gm, "done") == "done":
                            gm = None
                        if gp is not None and next(gp, "done") == "done":
                            gp = None
                    if l == 0:
                        conv_pump(0, 5)
                    if prev is not None and prev[0] > 0:
                        for _ in post_gens(prev, "late"):
                            pass
                    prev = (gi, blks)
                for _ in post_gens(prev, "all"):
                    pass
                if l == 0:
                    conv_pump(0, 1000)
                K.barrier()

            if not last:
                pblocks = list(range(post_lo, post_hi)) + [NKV, NKV + 1]
            else:
                pblocks = list(range(post_lo, post_hi))
            with ExitStack() as st:
                gm2 = [sb(f"gm2_{s}", [128, D], BF16, st) for s in range(2)]
                sh2 = [sb(f"sh2_{s}", [128, D], BF16, st) for s in range(2)]
                g2 = [sb(f"g2_{s}", [128, D], BF16, st) for s in range(2)]
                emit_mod(l, [(3, sh2, None), (4, gm2, P["n2g"]), (5, g2, None)])
                UTg = [sb(f"UTg{i}", [128, 8, 512], BF16, st) for i in range(2)]
                Vg = [sb(f"Vg{i}", [128, 4, D], BF16, st) for i in range(2)]
                wqj = [sb(f"wqj{i}", [128, 8, 128], BF16, st) for i in range(2)]
                keysT = sb("keysT", [128, 2048], BF16, st)
                Wsb = sb("Wsb", [128, 256, 128], BF16, st)
                hT2 = [sb(f"hT2_{i}", [128, 8, 256], BF16, st) for i in range(2)]
                qT = sb("qT", [128, 16, 256], BF16, st)
                xin1 = sb("xin1", [128, D], F32, st)
                hf2 = sb("hf2", [128, D], F32, st)
                Ssb = sb("Ssb", [128, 2048], F32, st)
                wrk = sb("wrk", [128, 2048], F32, st)
                sv = sb("sv", [128, 256], F32, st)
                si = sb("si", [128, 256], U32, st)
                sif = sb("sif", [128, 256], F32, st)
                fv = sb("fv", [128, 128], F32, st)
                fi = sb("fi", [128, 128], U32, st)
                fif = sb("fif", [128, 128], F32, st)
                faf = sb("faf", [128, 128], F32, st)
                fbf = sb("fbf", [128, 128], F32, st)
                If = sb("If", [128, 128], F32, st)
                Jf = sb("Jf", [128, 128], F32, st)
                gf = sb("gf", [128, 128], F32, st)
                zz = sb("zz", [128, 16], F32, st)
                ITb = [sb(f"ITb{i}", [128, 256], BF16, st) for i in range(2)]
                JTb = [sb(f"JTb{i}", [128, 256], BF16, st) for i in range(2)]
                gTb = [sb(f"gTb{i}", [128, 256], BF16, st) for i in range(2)]
                iob = sb("iob", [128, 128], BF16, st)
                Lb = [sb(f"Lb{i}", [128, 8, 128], BF16, st) for i in range(2)]
                Rb = [sb(f"Rb{i}", [128, 8, 128], BF16, st) for i in range(2)]
                Gs = [sb(f"Gs{i}", [128, 256], BF16, st) for i in range(3)]
                Hs = [sb(f"Hs{i}", [128, 256], BF16, st) for i in range(4)]
                tmpP = sb("tmpP", [128, 512], F32, st)

                wqsrc = P["wq"].rearrange("(c p) n -> p c n", p=128)
                K.dma("pool", keysT[:], P["keysT"][:, :])
                K.cp("dve", iob[:], iota128[:])
                utsrc = P["utb"].rearrange("(c p) n -> p c n", p=128)
                vsrc = P["vb"].rearrange("(j i) d -> i j d", i=128)

                def load_group(jg):
                    i = jg % 2
                    K.dma("sp", UTg[i][:], utsrc[:, :, jg * 512:(jg + 1) * 512], reads=[])
                    K.dma("sp", Vg[i][:], vsrc[:, jg * 4:(jg + 1) * 4, :], reads=[])

                def p1_gen(ti, blks):
                    par = ti % 2
                    nb = len(blks)
                    N = nb * 128
                    h2 = hT2[par]
                    for bi, b in enumerate(blks):
                        s = 1 if b >= NKV else 0
                        K.dma("sp", xin1[:], xdst[b * 128:(b + 1) * 128, :])
                        ss, rs = stat[:, 30:31], stat[:, 31:32]
                        K.act(junk[:], xin1[:], AF.Square, accum_out=ss, wk=["junk", "st30"])
                        K.act(rs, ss, AF.Ln, bias=EPS, scale=1.0 / D, rk=["st30"], wk=["st31"])
                        K.act(rs, rs, AF.Exp, scale=-0.5, rk=["st31"], wk=["st31"])
                        K.stt(hf2[:], xin1[:], rs, gm2[s][:], ALU.mult, ALU.mult, rk=["xin1", "st31", KN(gm2[s])])
                        K.tt("dve", hf2[:], hf2[:], sh2[s][:], ALU.add)
                        yield
                        transpose_to(hf2, 8, lambda c0, n, bi=bi: h2[:, c0:c0 + n, bi * 128:(bi + 1) * 128], banks=(4, 4))
                        yield
                    K.dma("pool", wqj[0][:], wqsrc[:, :, 0:128])
                    for j in range(16):
                        w = wqj[j % 2]
                        if j + 1 < 16:
                            K.dma("pool", wqj[(j + 1) % 2][:], wqsrc[:, :, (j + 1) * 128:(j + 2) * 128])
                        ps = R[4]
                        for c in range(8):
                            K.mm(ps[:, 0:N], w[:, c, :], h2[:, c, 0:N], start=(c == 0), stop=(c == 7))
                        K.cp("act", qT[:, j, 0:N], ps[:, 0:N], wk=[f"qT{j}"])
                        yield
                    for bi, b in enumerate(blks):
                        tsl = slice(bi * 128, (bi + 1) * 128)
                        for q4 in range(4):
                            ps = R[4]
                            for jj in range(4):
                                j = q4 * 4 + jj
                                K.mm(ps[:, jj * 128:(jj + 1) * 128], qT[:, j, tsl], keysT[:, j * 128:(j + 1) * 128],
                                     rk=[f"qT{j}", "keysT"])
                            K.cp("act", Ssb[:, q4 * 512:(q4 + 1) * 512], ps[:, :], wk=[f"S{q4}"])
                            yield

                        def top16_all(n, srcf, wkf, valf, idxf, key, srckeys):
                            for j in range(n):
                                K.dv("max", srckeys(j), [f"{key}{j}v0"], out=valf(j)[:, 0:8], in_=srcf(j))
                            yield
                            for j in range(n):
                                K.dv("max_index", srckeys(j) + [f"{key}{j}v0"], [f"{key}{j}i0"], out=idxf(j)[:, 0:8],
                                     in_max=valf(j)[:, 0:8], in_values=srcf(j))
                            yield
                            for j in range(n):
                                K.dv("match_replace", srckeys(j) + [f"{key}{j}v0"], [f"{key}{j}w"], out=wkf(j),
                                     in_to_replace=valf(j)[:, 0:8], in_values=srcf(j), imm_value=-1e30)
                            yield
                            for j in range(n):
                                K.dv("max", [f"{key}{j}w"], [f"{key}{j}v1"], out=valf(j)[:, 8:16], in_=wkf(j))
                            yield
                            for j in range(n):
                                K.dv("max_index", [f"{key}{j}w", f"{key}{j}v1"], [f"{key}{j}i1"], out=idxf(j)[:, 8:16],
                                     in_max=valf(j)[:, 8:16], in_values=wkf(j))
                            yield
                        yield from top16_all(16, lambda j: Ssb[:, j * 128:(j + 1) * 128], lambda j: wrk[:, j * 128:(j + 1) * 128],
                                             lambda j: sv[:, j * 16:(j + 1) * 16], lambda j: si[:, j * 16:(j + 1) * 16], "t",
                                             lambda j: [f"S{j // 4}"])
                        SVK = [f"t{j}v{u}" for j in range(16) for u in range(2)]
                        SIK = [f"t{j}i{u}" for j in range(16) for u in range(2)]
                        TWK = [f"t{j}w" for j in range(16)]
                        sv4 = sv[:].rearrange("p (h t a) -> p h t a", h=8, t=2)
                        K.tt("dve", Ssb[:].rearrange("p (h a b) -> p h a b", h=8, a=16),
                             sv4[:, :, 0, :].unsqueeze(3).to_broadcast([128, 8, 16, 16]),
                             sv4[:, :, 1, :].unsqueeze(2).to_broadcast([128, 8, 16, 16]), ALU.add, rk=SVK, wk=["cand"])
                        yield from top16_all(8, lambda h: Ssb[:, h * 256:(h + 1) * 256], lambda h: wrk[:, h * 256:(h + 1) * 256],
                                             lambda h: fv[:, h * 16:(h + 1) * 16], lambda h: fi[:, h * 16:(h + 1) * 16], "f",
                                             lambda h: ["cand", "S0", "S1", "S2", "S3"] + TWK)
                        FVK = [f"f{h}v{u}" for h in range(8) for u in range(2)]
                        FIK = [f"f{h}i{u}" for h in range(8) for u in range(2)]
                        FWK = [f"f{h}w" for h in range(8)]
                        fv3 = fv[:].rearrange("p (h k) -> p h k", h=8)
                        gf3 = gf[:].rearrange("p (h k) -> p h k", h=8)
                        K.tt("dve", gf3, fv3, fv3[:, :, 0:1].to_broadcast([128, 8, 16]), ALU.subtract, rk=FVK, wk=["gf"])
                        K.act(gf[:], gf[:], AF.Exp)
                        K.cp("dve", fif[:], fi[:], rk=FIK)
                        K.cp("dve", sif[:], si[:], rk=SIK)
                        yield
                        K.dv("tensor_reduce", ["gf"], ["zz"], out=zz[:, 0:8], in_=gf3, axis=AX.X, op=ALU.add)
                        ge3 = wrk[:, 0:1920].rearrange("p (n m) -> p n m", m=15)
                        K.tt("dve", ge3, fif[:].unsqueeze(2).to_broadcast([128, 128, 15]),
                             thr16[:, 1:16].unsqueeze(1).to_broadcast([128, 128, 15]), ALU.is_ge,
                             rk=["fif", "thr16"] + FWK, wk=["eq"])
                        K.dv("reciprocal", ["zz"], ["zz2"], out=zz[:, 8:16], in_=zz[:, 0:8])
                        K.dv("tensor_reduce", ["eq"], ["faf"], out=faf[:], in_=ge3, axis=AX.X, op=ALU.add)
                        K.tt("dve", gf3, gf3, zz[:, 8:16].unsqueeze(2).to_broadcast([128, 8, 16]), ALU.mult, rk=["gf", "zz2"])
                        K.stt(fbf[:], faf[:], -16.0, fif[:], ALU.mult, ALU.add)
                        yield
                        sif4 = sif[:].rearrange("p (h t a) -> p h t a", h=8, t=2)
                        eq4 = wrk[:].rearrange("p (h k a) -> p h k a", h=8, k=16)
                        io4 = iota16[:].unsqueeze(1).unsqueeze(1).to_broadcast([128, 8, 16, 16])
                        for (ff, t_, dst) in ((faf, 0, If), (fbf, 1, Jf)):
                            K.tt("dve", eq4, io4, ff[:].rearrange("p (h k) -> p h k", h=8).unsqueeze(3).to_broadcast([128, 8, 16, 16]),
                                 ALU.is_equal, rk=["iota16", KN(ff), "faf"], wk=["eq"])
                            K.tt("dve", eq4, eq4, sif4[:, :, t_, :].unsqueeze(2).to_broadcast([128, 8, 16, 16]), ALU.mult,
                                 rk=["eq", "sif"], wk=["eq"])
                            K.dv("tensor_reduce", ["eq"], [KN(dst)], out=dst[:].rearrange("p (h k) -> p h k", h=8), in_=eq4,
                                 axis=AX.X, op=ALU.add)
                            yield
                        ps = R[4]
                        K.tr(ps[:, 0:128], If[:], ident[:])
                        K.tr(ps[:, 128:256], Jf[:], ident[:])
                        K.tr(ps[:, 256:384], gf[:], ident[:])
                        K.cp("act", ITb[par][:, tsl], ps[:, 0:128], wk=[f"IT{par}{bi}"])
                        K.cp("act", JTb[par][:, tsl], ps[:, 128:256], wk=[f"JT{par}{bi}"])
                        K.cp("act", gTb[par][:, tsl], ps[:, 256:384], wk=[f"gT{par}{bi}"])
                        yield

                def p2(ti, blks):
                    par = ti % 2
                    N = len(blks) * 128
                    for gidx, t0 in enumerate(range(0, N, 4)):
                        bi = t0 // 128
                        pp = gidx % 4
                        for u in range(4):
                            t = t0 + u
                            K.ts(Lb[pp // 2][:, (pp % 2) * 4 + u, :], iob[:], ITb[par][:, t:t + 1], None,
                                 ALU.is_equal, rk=["iob", f"IT{par}{bi}"], wk=[f"L{pp}"])
                            K.ts(Rb[pp // 2][:, (pp % 2) * 4 + u, :], iob[:], JTb[par][:, t:t + 1], None,
                                 ALU.is_equal, rk=["iob", f"JT{par}{bi}"], wk=[f"Rr{pp}"])
                        L4 = Lb[pp // 2][:, (pp % 2) * 4:(pp % 2) * 4 + 4, :]
                        K.tt("pool", L4, L4, gTb[par][:, t0:t0 + 4].unsqueeze(2).to_broadcast([128, 4, 128]), ALU.mult,
                             rk=[f"L{pp}", f"gT{par}{bi}"], wk=[f"L{pp}"])
                        ps = R[4 + gidx % 4]
                        for u in range(4):
                            K.mm(ps[:, u * 128:(u + 1) * 128], Lb[pp // 2][:, (pp % 2) * 4 + u, :], Rb[pp // 2][:, (pp % 2) * 4 + u, :],
                                 rk=[f"L{pp}", f"Rr{pp}"])
                        K.cp("act", Wsb[:, t0:t0 + 4, :], ps[:, :].rearrange("p (t j) -> p t j", t=4), wk=["Wsb"])

                def p3_gen(ti, blks):
                    par = ti % 2
                    nb = len(blks)
                    N = nb * 128
                    h2 = hT2[par]

                    def emitA(j):
                        i, jj = (j // 4) % 2, j % 4
                        A = R[5 + j % 3][:, 0:N]
                        for c in range(8):
                            K.mm(A, UTg[i][:, c, jj * 128:(jj + 1) * 128], h2[:, c, 0:N], start=(c == 0), stop=(c == 7))
                    load_group(0)
                    load_group(1)
                    emitA(0)
                    emitA(1)
                    for j in range(128):
                        jg, jj = j // 4, j % 4
                        i = jg % 2
                        if j + 2 < 128:
                            emitA(j + 2)
                        A = R[5 + j % 3][:, 0:N]
                        G = Gs[j % 3]
                        K.act(G[:, 0:N], A, AF.Gelu_apprx_tanh)
                        H = Hs[j % 4]
                        K.tt("pool", H[:, 0:N], G[:, 0:N], Wsb[:, 0:N, j], ALU.mult)
                        for bi in range(nb):
                            for hh in range(2):
                                K.mm(R[bi * 2 + hh][:, :], H[:, bi * 128:(bi + 1) * 128], Vg[i][:, jj, hh * 512:(hh + 1) * 512],
                                     start=(j == 0), stop=(j == 127))
                        if jj == 3 and jg + 2 < 32:
                            load_group(jg + 2)
                        yield

                def p4(ti, blks):
                    for bi, b in enumerate(blks):
                        s = 1 if b >= NKV else 0
                        K.dma("sp", xin1[:], xdst[b * 128:(b + 1) * 128, :])
                        for hh in range(2):
                            hs = slice(hh * 512, (hh + 1) * 512)
                            K.tt("dve", tmpP[:], R[bi * 2 + hh][:, :], g2[s][:, hs], ALU.mult)
                            K.tt("pool", hf2[:, hs], tmpP[:], xin1[:, hs], ALU.add, rk=["tmpP", "xin1"], wk=["hf2"])
                        if last:
                            ss, rs = stat[:, 34:35], stat[:, 35:36]
                            K.act(junk[:], hf2[:], AF.Square, accum_out=ss, rk=["hf2"], wk=["junk", "st34"])
                            K.act(rs, ss, AF.Ln, bias=EPS, scale=1.0 / D, rk=["st34"], wk=["st35"])
                            K.act(rs, rs, AF.Exp, scale=-0.5, rk=["st35"], wk=["st35"])
                            K.stt(hf2[:], hf2[:], rs, fg_bc[:], ALU.mult, ALU.mult, rk=["hf2", "st35", "fg_bc"], wk=["hf2"])
                            ob = b - post_lo
                            K.dma("sp", out_d[ob * 128:(ob + 1) * 128, :], hf2[:], reads=["hf2"], writes=[f"o{b}"])
                        else:
                            K.dma("sp", x1[b * 128:(b + 1) * 128, :], hf2[:], reads=["hf2"], writes=[f"x1_{b}"])

                tiles = [pblocks[i_:i_ + 2] for i_ in range(0, len(pblocks), 2)]
                for _ in p1_gen(0, tiles[0]):
                    pass
                for ti, blks in enumerate(tiles):
                    p2(ti, blks)
                    g1 = p1_gen(ti + 1, tiles[ti + 1]) if ti + 1 < len(tiles) else None
                    for step, _ in enumerate(p3_gen(ti, blks)):
                        if g1 is not None and step % 2 == 1:
                            if next(g1, "done") == "done":
                                g1 = None
                    if g1 is not None:
                        for _ in g1:
                            pass
                    p4(ti, blks)
                    if l + 1 < DEPTH:
                        conv_pump(l + 1, 3)
                if l + 1 < DEPTH:
                    conv_pump(l + 1, 1000)
                K.barrier()
        K.barrier()
        print("instruction counts:", K.ninst, "sem incs:", K.ninc)
        waited = K.waited
    return nc, waited


def _host_inputs(NB, x, c, ctx, c_ctx, w_ada, b_ada, norm1_g, norm2_g, w_in, conv_w, sgu_norm_g, sgu_w, sgu_b,
                 attn_sink, mix_norm_g, w_out, peer_wq, peer_keys, peer_u, peer_v, final_g):
    f32 = np.float32
    B, S, _ = x.shape
    HALF = NB * 128
    assert S == 2 * HALF
    NKV = NB + 4
    NLAT = NKV * 128
    NTOK = NLAT + 256
    NBLK = NKV + 2
    DEPTH = w_in.shape[0]
    cols = list(range(0, 1280)) + list(range(1920, 2048))
    qcols = []
    for cch in range(4):
        for h in (cch, 4 + cch):
            qcols += [1280 + h * 64 + d for d in range(64)]
    kcols = [1792 + i for i in range(128)]

    def perm(d):
        return d + 16 if (d % 32) < 16 else d - 16
    qpcols = [1280 + ((q - 1280) // 64) * 64 + perm((q - 1280) % 64) for q in qcols]
    kpcols = [1792 + ((k - 1792) // 64) * 64 + perm((k - 1792) % 64) for k in kcols]
    colidx = np.array(cols + qcols + kcols + qpcols + kpcols)
    assert len(colidx) == NCOLD
    shared = {}
    shared["ident"] = np.eye(128, dtype=f32)
    shared["iota128"] = np.tile(np.arange(128, dtype=f32)[None, :], (128, 1))
    shared["iota16"] = np.tile(np.arange(16, dtype=f32)[None, :], (128, 1))
    kk = np.arange(128)[:, None]
    qq = np.arange(128)[None, :]
    shared["masks"] = np.concatenate([(kk >= qq), (kk <= qq)], axis=1).astype(f32)
    shared["final_g"] = np.ascontiguousarray(final_g.reshape(1, D), dtype=f32)
    for l in range(DEPTH):
        shared[f"wada{l}"] = np.ascontiguousarray(w_ada[l])
        shared[f"bada{l}"] = np.ascontiguousarray(b_ada[l].reshape(1, -1))
        shared[f"n1g{l}"] = np.ascontiguousarray(norm1_g[l].reshape(1, -1))
        shared[f"n2g{l}"] = np.ascontiguousarray(norm2_g[l].reshape(1, -1))
        shared[f"wd{l}"] = np.ascontiguousarray(w_in[l][:, colidx])
        shared[f"cw{l}"] = np.ascontiguousarray(conv_w[l].reshape(3, 2, 128).transpose(2, 1, 0).reshape(128, 6))
        shared[f"sgg{l}"] = np.ascontiguousarray(sgu_norm_g[l].reshape(1, -1))
        shared[f"wsT{l}"] = np.ascontiguousarray(sgu_w[l].transpose(2, 0, 1).reshape(128, 512))
        shared[f"sbT{l}"] = np.ascontiguousarray(sgu_b[l].T)
        shared[f"sink{l}"] = np.ascontiguousarray(attn_sink[l].reshape(1, 8))
        shared[f"gmix{l}"] = np.ascontiguousarray(mix_norm_g[l].reshape(1, -1))
        shared[f"gmT{l}"] = np.ascontiguousarray(mix_norm_g[l].reshape(8, 128).T)
        shared[f"wo{l}"] = np.ascontiguousarray(w_out[l])
        shared[f"wq{l}"] = np.ascontiguousarray(peer_wq[l])
        shared[f"keysT{l}"] = np.ascontiguousarray(peer_keys[l].reshape(16, 128, 128).transpose(2, 0, 1).reshape(128, 2048))
        shared[f"ut{l}"] = np.ascontiguousarray(peer_u[l].reshape(128, 128, D).transpose(2, 1, 0).reshape(D, 16384))
        shared[f"vp{l}"] = np.ascontiguousarray(peer_v[l].reshape(128, 128, D).transpose(1, 0, 2).reshape(16384, D))
    inv = (10000.0 ** (-np.arange(16, dtype=np.float32) / 16)).astype(f32)
    sign = np.where((np.arange(64) % 32) < 16, -1.0, 1.0).astype(f32)
    in_maps = []
    for core in range(2 * B):
        b, hf = core // 2, core % 2
        start = hf * HALF
        m = dict(shared)
        tpos = np.arange(start - 256, start + HALF + 256)
        valid = ((tpos >= 0) & (tpos < S))
        xe = np.zeros((NLAT, D), f32)
        xe[valid] = x[b, tpos[valid]]
        m["x_ext"] = xe
        m["ctx_in"] = np.ascontiguousarray(ctx[b])
        cT = np.zeros((128, 8, 2), f32)
        cT[:, :, 0] = c[b].reshape(8, 128).T
        cT[:, :, 1] = c_ctx.reshape(8, 128).T
        m["cT"] = cT.reshape(128, 16)
        vfull = np.concatenate([valid.astype(f32), np.ones(256, f32)])
        m["validT"] = np.ascontiguousarray(vfull.reshape(NBLK, 128).T)
        tp = np.clip(tpos, 0, S - 1)
        row = (tp // 64).astype(f32)
        col = (tp % 64).astype(f32)
        ar = row[:, None] * inv[None, :]
        ac = col[:, None] * inv[None, :]
        ang = np.concatenate([ar, ar, ac, ac], axis=-1).astype(f32)
        cos = np.concatenate([np.cos(ang), np.ones((256, 64), f32)], axis=0)
        sin = np.concatenate([np.sin(ang) * sign[None, :], np.zeros((256, 64), f32)], axis=0)
        m["cosT"] = np.ascontiguousarray(np.tile(cos.T, (2, 1)), dtype=f32)
        m["sinT"] = np.ascontiguousarray(np.tile(sin.T, (2, 1)), dtype=f32)
        in_maps.append(m)
    return in_maps


_CACHE = {}


def kernel(**inputs):
    inputs = {k: np.asarray(v) for k, v in inputs.items()}
    x = inputs["x"]
    B, S, _ = x.shape
    NB = S // 256
    if NB not in _CACHE:
        _CACHE[NB] = build_program(NB, DEPTH=inputs["w_in"].shape[0])
    nc = _CACHE[NB]
    in_maps = _host_inputs(NB, **inputs)
    res = run_bass_kernel_spmd(nc, in_maps, core_ids=list(range(2 * B)))
    out = np.zeros((B, S, D), np.float32)
    for core in range(2 * B):
        b, hf = core // 2, core % 2
        out[b, hf * NB * 128:(hf + 1) * NB * 128] = res.results[core]["out"]
    return out
```

```python
import numpy as np
from contextlib import ExitStack
import concourse.bass as bass
import concourse.mybir as mybir
from concourse.bass_utils import run_bass_kernel_spmd

F32 = mybir.dt.float32
BF16 = mybir.dt.bfloat16
U32 = mybir.dt.uint32
AF = mybir.ActivationFunctionType
ALU = mybir.AluOpType
AX = mybir.AxisListType

D = 1024
EPS = 1e-6
NCOLD = 2688
C_BG, C_CG, C_XI, C_SGU, C_V, C_Q, C_K, C_QP, C_KP = 0, 256, 512, 768, 1280, 1408, 1920, 2048, 2560
EPOCH = 20000


def KN(t):
    return t.name.split("__")[0]


class Sched:
    ENG = ("pe", "dve", "act", "pool", "sp")

    def __init__(self, nc, es, dma_slots, needed=None):
        self.nc = nc
        self.es = es
        self.eng = {"pe": nc.tensor, "dve": nc.vector, "act": nc.scalar, "pool": nc.gpsimd, "sp": nc.sync}
        self.needed = needed
        self.waited = set()
        self.sem = {}
        self.semname = {}
        self.cnt = {e: 0 for e in self.ENG}
        self.seq = {e: 0 for e in self.ENG}
        self.epoch = {e: -1 for e in self.ENG}
        self.semval = {}
        self.known = {e: {} for e in self.ENG}
        self.lastw = {}
        self.readers = {}
        self.ninst = {e: 0 for e in self.ENG}
        self.ninc = 0
        for e in self.ENG:
            self._new_epoch(e)
        self.dsem = {q: [es.enter_context(nc.semaphore(f"d_{q}{i}")) for i in range(n)] for q, n in dma_slots.items()}
        self.dcnt = {q: [0] * n for q, n in dma_slots.items()}
        self.dnext = {q: 0 for q in dma_slots}

    def _new_epoch(self, e):
        self.epoch[e] += 1
        name = f"s_{e}{self.epoch[e]}"
        self.sem[e] = self.es.enter_context(self.nc.semaphore(name))
        self.semname[e] = name
        self.cnt[e] = 0

    def _wait(self, e, prod):
        kind, pid, val = prod[0], prod[1], prod[2]
        k = self.known[e]
        if k.get((kind, pid), 0) >= val:
            return
        k[(kind, pid)] = val
        if kind == "c":
            self.waited.add((pid, val))
            semh, sval = self.semval[(pid, val)] if self.needed is not None else self.semval_all[(pid, val)]
            self.eng[e].wait_ge(semh, sval)
        else:
            self.eng[e].wait_ge(prod[4], val)
        self.ninst[e] += 1

    semval_all = None

    def _deps(self, e, reads, writes, is_dma):
        deps = []
        for k in reads:
            w = self.lastw.get(k)
            if w is not None:
                if is_dma or not (w[3] == e and e == "pe"):
                    deps.append(w)
        for k in writes:
            w = self.lastw.get(k)
            if w is not None and (is_dma or w[3] != e):
                deps.append(w)
            for r in self.readers.get(k, {}).values():
                if is_dma or r[3] != e:
                    deps.append(r)
        for d in deps:
            self._wait(e, d)

    def _record(self, prod, reads, writes):
        for k in writes:
            self.lastw[k] = prod
            self.readers[k] = {}
        for k in reads:
            self.readers.setdefault(k, {})[(prod[0], prod[1])] = prod

    def op(self, e, fn, reads, writes):
        reads = [r for r in reads if r is not None]
        self._deps(e, reads, writes, False)
        self.seq[e] += 1
        me = (e, self.seq[e])
        inst = fn(self.eng[e])
        self.ninst[e] += 1
        if self.needed is None or me in self.needed:
            if self.cnt[e] >= EPOCH:
                self._new_epoch(e)
            self.cnt[e] += 1
            inst.then_inc(self.sem[e], 1)
            self.ninc += 1
            if self.needed is None:
                if self.semval_all is None:
                    self.semval_all = {}
                self.semval_all[me] = (self.sem[e], self.cnt[e])
            else:
                self.semval[me] = (self.sem[e], self.cnt[e])
        prod = ("c", e, self.seq[e], e)
        self._record(prod, reads, writes)
        return prod

    def dma(self, q, out, in_, reads=None, writes=None, slow=False):
        reads = [KN(in_)] if reads is None else reads
        writes = [KN(out)] if writes is None else writes
        slot = self.dnext[q]
        self.dnext[q] = (slot + 1) % len(self.dsem[q])
        semh = self.dsem[q][slot]
        name = f"d_{q}{slot}"
        if self.dcnt[q][slot] > 0:
            self._wait(q, ("d", name, self.dcnt[q][slot] * 16, "dma", semh))
        self._deps(q, reads, writes, True)
        self.dcnt[q][slot] += 1
        kw = {"allow_slow_non_contiguous": True} if slow else {}
        self.eng[q].dma_start(out=out, in_=in_, **kw).then_inc(semh, 16)
        self.ninst[q] += 1
        prod = ("d", name, self.dcnt[q][slot] * 16, "dma", semh)
        self._record(prod, reads, writes)
        return prod

    def barrier(self):
        prods = []
        for e in self.ENG:
            if self.seq[e] > 0:
                prods.append(("c", e, self.seq[e], e))
        for q in self.dsem:
            for i, s in enumerate(self.dsem[q]):
                if self.dcnt[q][i] > 0:
                    prods.append(("d", f"d_{q}{i}", self.dcnt[q][i] * 16, "dma", s))
        for e in self.ENG:
            for p in prods:
                if p[3] != e:
                    self._wait(e, p)
        self.lastw.clear()
        self.readers.clear()

    @staticmethod
    def _k(*aps):
        return [KN(a) for a in aps if hasattr(a, "name")]

    def mm(self, out, lhsT, rhs, start=True, stop=True, rk=None, wk=None):
        return self.op("pe", lambda e: e.matmul(out, lhsT=lhsT, rhs=rhs, start=start, stop=stop),
                       self._k(lhsT, rhs) if rk is None else rk, self._k(out) if wk is None else wk)

    def tr(self, out, in_, ident, rk=None, wk=None):
        return self.op("pe", lambda e: e.transpose(out=out, in_=in_, identity=ident),
                       self._k(in_, ident) if rk is None else rk, self._k(out) if wk is None else wk)

    def act(self, out, in_, func, bias=None, scale=None, accum_out=None, rk=None, wk=None):
        kw = {}
        if bias is not None:
            kw["bias"] = bias
        if scale is not None:
            kw["scale"] = scale
        if accum_out is not None:
            kw["accum_out"] = accum_out
        r = self._k(in_, bias, scale) if rk is None else rk
        w = self._k(out, accum_out) if wk is None else wk
        return self.op("act", lambda e: e.activation(out=out, in_=in_, func=func, **kw), r, w)

    def tt(self, e, out, in0, in1, op, rk=None, wk=None):
        return self.op(e, lambda g: g.tensor_tensor(out=out, in0=in0, in1=in1, op=op),
                       self._k(in0, in1) if rk is None else rk, self._k(out) if wk is None else wk)

    def ts(self, out, in0, s1, s2=None, op0=ALU.mult, op1=None, rk=None, wk=None):
        kw = {} if op1 is None else {"op1": op1}
        return self.op("dve", lambda g: g.tensor_scalar(out=out, in0=in0, scalar1=s1, scalar2=s2, op0=op0, **kw),
                       self._k(in0, s1, s2) if rk is None else rk, self._k(out) if wk is None else wk)

    def stt(self, out, in0, scalar, in1, op0, op1, rk=None, wk=None):
        return self.op("dve", lambda g: g.scalar_tensor_tensor(out=out, in0=in0, scalar=scalar, in1=in1, op0=op0, op1=op1),
                       self._k(in0, scalar, in1) if rk is None else rk, self._k(out) if wk is None else wk)

    def cp(self, e, out, in_, rk=None, wk=None):
        r = self._k(in_) if rk is None else rk
        w = self._k(out) if wk is None else wk
        if e == "act":
            return self.op("act", lambda g: g.activation(out=out, in_=in_, func=AF.Copy), r, w)
        return self.op(e, lambda g: g.tensor_copy(out=out, in_=in_), r, w)

    def dv(self, name, reads, writes, **kw):
        return self.op("dve", lambda g: getattr(g, name)(**kw), reads, writes)


def build_program(NB, DEPTH=2):
    needed = _build(NB, DEPTH, None)[1]
    return _build(NB, DEPTH, needed)[0]


def _build(NB, DEPTH, needed):
    NKV = NB + 4
    NLAT = NKV * 128
    NTOK = NLAT + 256
    NBLK = NKV + 2
    ZW = NTOK + 4
    nc = bass.Bass("TRN2", target_bir_lowering=False)

    def din(name, shape, dt=F32):
        return nc.dram_tensor(name, list(shape), dt, kind="ExternalInput").ap()

    def dscr(name, shape, dt=F32):
        return nc.dram_tensor(name, list(shape), dt, kind="Internal").ap()

    x_ext = din("x_ext", [NLAT, D])
    ctx_in = din("ctx_in", [256, D])
    cT_in = din("cT", [128, 16])
    validT_in = din("validT", [128, NBLK])
    cos_in = din("cosT", [128, NTOK])
    sin_in = din("sinT", [128, NTOK])
    ident_in = din("ident", [128, 128])
    iota_in = din("iota128", [128, 128])
    iota16_in = din("iota16", [128, 16])
    masks_in = din("masks", [128, 256])
    fg_in = din("final_g", [1, D])
    L = []
    for l in range(DEPTH):
        L.append(dict(
            wada=din(f"wada{l}", [D, 6 * D]), bada=din(f"bada{l}", [1, 6 * D]),
            n1g=din(f"n1g{l}", [1, D]), n2g=din(f"n2g{l}", [1, D]),
            wd=din(f"wd{l}", [D, NCOLD]), cw=din(f"cw{l}", [128, 6]),
            sgg=din(f"sgg{l}", [1, 256]), wsT=din(f"wsT{l}", [128, 512]), sbT=din(f"sbT{l}", [128, 4]),
            sink=din(f"sink{l}", [1, 8]), gmix=din(f"gmix{l}", [1, D]), gmT=din(f"gmT{l}", [128, 8]),
            wo=din(f"wo{l}", [D, D]), wq=din(f"wq{l}", [D, 2048]), keysT=din(f"keysT{l}", [128, 2048]),
            ut=din(f"ut{l}", [D, 16384]), vp=din(f"vp{l}", [16384, D]),
            utb=dscr(f"utb{l}", [D, 16384], BF16), vb=dscr(f"vb{l}", [16384, D], BF16),
            wqb=dscr(f"wqb{l}", [D, 2048], BF16),
        ))
    out_d = nc.dram_tensor("out", [NB * 128, D], F32, kind="ExternalOutput").ap()
    xmid = [dscr(f"xmid{l}", [NBLK * 128, D]) for l in range(DEPTH)]
    x1 = dscr("x1", [NBLK * 128, D])

    es = ExitStack()
    with es:
        K = Sched(nc, es, {"sp": 16, "act": 4, "pool": 20}, needed)

        uid = [0]

        def sb(name, shape, dt=F32, st=None):
            uid[0] += 1
            return (st or es).enter_context(nc.sbuf_tensor(f"{name}__{uid[0]}", list(shape), dt))

        R = [es.enter_context(nc.psum_tensor(f"R{i}", [128, 512], F32)) for i in range(8)]
        ident = sb("ident", [128, 128])
        iota128 = sb("iota128", [128, 128])
        iota16 = sb("iota16", [128, 16])
        masks = sb("masks", [128, 256], BF16)
        validT = sb("validT", [128, NBLK])
        cT = sb("cT", [128, 16])
        scT = sb("scT", [128, 16])
        ones_bf = sb("ones_bf", [128, 128], BF16)
        ones_f = sb("ones_f", [1, 128])
        fg_bc = sb("fg_bc", [128, D])
        stat = sb("stat", [128, 64])
        junk = sb("junk", [128, D], BF16)

        K.dma("sp", ident[:], ident_in[:, :])
        K.dma("sp", iota128[:], iota_in[:, :])
        K.dma("sp", iota16[:], iota16_in[:, :])
        K.dma("pool", masks[:], masks_in[:, :])
        K.dma("sp", validT[:], validT_in[:, :])
        K.dma("sp", cT[:], cT_in[:, :])
        K.dma("sp", fg_bc[:], fg_in.partition_broadcast(128))
        K.op("dve", lambda g: g.memset(ones_bf[:], 1.0), [], ["ones_bf"])
        K.op("dve", lambda g: g.memset(ones_f[:], 1.0), [], ["ones_f"])
        K.act(scT[:], cT[:], AF.Silu)
        thr16 = sb("thr16", [128, 16])
        K.ts(thr16[:], iota16[:], 16.0, None, ALU.mult)

        conv_jobs = []
        for l in range(DEPTH):
            jobs = []
            for r in range(8):
                for cc in range(4):
                    jobs.append((L[l]["utb"][r * 128:(r + 1) * 128, cc * 4096:(cc + 1) * 4096],
                                 L[l]["ut"][r * 128:(r + 1) * 128, cc * 4096:(cc + 1) * 4096]))
            for r in range(8):
                jobs.insert(r, (L[l]["wqb"][r * 128:(r + 1) * 128, :], L[l]["wq"][r * 128:(r + 1) * 128, :]))
            vsrc = L[l]["vp"].rearrange("(a p) d -> p a d", p=128)
            vdst = L[l]["vb"].rearrange("(a p) d -> p a d", p=128)
            for a0 in range(0, 128, 8):
                jobs.append((vdst[:, a0:a0 + 8, :], vsrc[:, a0:a0 + 8, :]))
            conv_jobs.append(jobs)

        def conv_pump(l, n):
            for _ in range(n):
                if conv_jobs[l]:
                    o, i = conv_jobs[l].pop(0)
                    K.dma("pool", o, i, reads=[], writes=[f"cv{l}"])

        def rstd_from_ss(dst, ss, n):
            K.act(dst, ss, AF.Ln, bias=EPS, scale=1.0 / n)
            K.act(dst, dst, AF.Exp, scale=-0.5)

        def emit_mod(l, specs):
            with ExitStack() as st:
                wa = [sb(f"wa{i}", [128, 8, 256], st=st) for i in range(2)]
                ba = [sb(f"ba{i}", [1, 256], st=st) for i in range(2)]
                screp = [sb(f"screp{i}", [128, 8, 128], st=st) for i in range(2)]
                gb = sb("gb_bc", [128, D], st=st)
                scv = scT[:].rearrange("p (c r) -> p c r", r=2)
                for s_ in range(2):
                    K.cp("dve", screp[s_][:], scv[:, :, s_:s_ + 1].to_broadcast([128, 8, 128]))
                wsrc = L[l]["wada"].rearrange("(c p) n -> p c n", p=128)
                n = 0
                for (chunk, dsts, gd) in specs:
                    if gd is not None:
                        K.dma("sp", gb[:], gd.partition_broadcast(128))
                    for q in range(4):
                        col = chunk * D + q * 256
                        w = wa[n % 2]
                        b_ = ba[n % 2]
                        K.dma("sp", w[:], wsrc[:, :, col:col + 256])
                        K.dma("sp", b_[:], L[l]["bada"][:, col:col + 256])
                        for s_ in range(2):
                            ps = R[(2 * n + s_) % 4]
                            for c in range(8):
                                K.mm(ps[:, 0:256], screp[s_][:, c, :], w[:, c, :], start=(c == 0), stop=False)
                            K.mm(ps[:, 0:256], ones_f[:, :], b_[:, :], start=False, stop=True)
                            d_ = dsts[s_][:, q * 256:(q + 1) * 256]
                            if gd is None:
                                K.cp("act", d_, ps[:, 0:256])
                            else:
                                K.stt(d_, ps[:, 0:256], 1.0, gb[:, q * 256:(q + 1) * 256], ALU.add, ALU.mult)
                        n += 1
                K.barrier()

        def norm_mod(xt, hf, gm, sh, vcol, sc):
            ss, rs = stat[:, sc:sc + 1], stat[:, sc + 1:sc + 2]
            K.act(junk[:], xt, AF.Square, accum_out=ss)
            rstd_from_ss(rs, ss, D)
            if vcol is not None:
                K.tt("dve", rs, rs, vcol, ALU.mult)
                K.stt(hf, xt, rs, gm, ALU.mult, ALU.mult)
                K.stt(hf, sh, vcol, hf, ALU.mult, ALU.add)
            else:
                K.stt(hf, xt, rs, gm, ALU.mult, ALU.mult)
                K.tt("dve", hf, hf, sh, ALU.add)

        def transpose_to(hf, nch, dst_fn, banks=(0, 1)):
            for c0 in range(0, nch, 4):
                n = min(4, nch - c0)
                ps = R[banks[(c0 // 4) % 2]]
                for k in range(n):
                    K.tr(ps[:, k * 128:(k + 1) * 128], hf[:, (c0 + k) * 128:(c0 + k + 1) * 128], ident[:])
                K.cp("act", dst_fn(c0, n), ps[:, 0:n * 128].rearrange("p (c t) -> p c t", c=n))

        zD = dscr("zD", [128, 2, ZW], BF16)
        bgD = dscr("bgD", [128, 2, ZW], BF16)
        for l in range(DEPTH):
            P = L[l]
            last = (l == DEPTH - 1)
            lat_lo = 0 if l == 0 else 1
            lat_hi = NKV - lat_lo
            post_lo, post_hi = lat_lo + 1, lat_hi - 1
            xsrc_lat = x_ext if l == 0 else x1
            xdst = xmid[l]

            def xrows(b, l=l, xsrc_lat=xsrc_lat):
                if b >= NKV:
                    return (ctx_in[(b - NKV) * 128:(b - NKV + 1) * 128, :] if l == 0 else x1[b * 128:(b + 1) * 128, :])
                return xsrc_lat[b * 128:(b + 1) * 128, :]

            with ExitStack() as st:
                gm1 = [sb(f"gm1_{s}", [128, D], F32, st) for s in range(2)]
                sh1 = [sb(f"sh1_{s}", [128, D], F32, st) for s in range(2)]
                g1 = [sb(f"g1_{s}", [128, D], F32, st) for s in range(2)]
                emit_mod(l, [(0, sh1, None), (1, gm1, P["n1g"]), (2, g1, None)])
                wd = sb("wd", [128, 8, NCOLD], BF16, st)
                wo = sb("wo", [128, 8, D], BF16, st)
                cosg = sb("cosg", [128, 512], BF16, st)
                sing = sb("sing", [128, 512], BF16, st)
                KT = sb("KT", [128, NTOK], BF16, st)
                Va = sb("Va", [128, NBLK, 2, 65], BF16, st)
                zg = sb("zg", [128, 2, 512], BF16, st)
                bgg = sb("bgg", [128, 2, 512], BF16, st)
                zb = sb("zb", [128, 2, 130], BF16, st)
                bgb = sb("bgb", [128, 2, 128], BF16, st)
                zpad = sb("zpad", [128, 2, 1], BF16, st)
                QT = [sb(f"QT{i}", [128, 4, 512], BF16, st) for i in range(2)]
                ysgT = [sb(f"ysgT{i}", [128, 2, 512], BF16, st) for i in range(2)]
                hTg = sb("hTg", [128, 8, 512], BF16, st)
                xt = [sb(f"xt{i}", [128, D], F32, st) for i in range(2)]
                hf = sb("hf", [128, D], F32, st)
                gmix_bc = sb("gmix_bc", [128, D], F32, st)
                gmT = sb("gmT", [128, 8], F32, st)
                cw = sb("cw", [128, 6], F32, st)
                sgg_bc = sb("sgg_bc", [128, 256], F32, st)
                wsT = sb("wsT", [128, 512], BF16, st)
                sbT = sb("sbT", [128, 4], F32, st)
                esink = sb("esink", [128, 8], F32, st)
                tmpA = sb("tmpA", [128, 512], F32, st)
                tmpB = sb("tmpB", [128, 512], F32, st)
                cgt = sb("cgt", [128, 512], F32, st)
                zs = sb("zs", [128, 512], F32, st)
                vln = sb("vln", [128, 256], F32, st)
                vlb = sb("vlb", [128, 256], BF16, st)
                ysg = sb("ysg", [128, 256], F32, st)
                bnst = sb("bnst", [128, 8], F32, st)
                PT = sb("PT", [128, 5, 512], BF16, st)
                att = sb("att", [128, 512], F32, st)
                yc = sb("yc", [128, 2, 128], F32, st)
                ycq = sb("ycq", [128, 2, 128], BF16, st)
                ca = sb("ca", [128, 128], F32, st)
                rsb = sb("rsb", [128, 128], F32, st)
                ynT = sb("ynT", [128, 8, 128], BF16, st)
                xr = sb("xr", [128, D], F32, st)
                xn = sb("xn", [128, D], F32, st)

                wdsrc = P["wd"].rearrange("(c p) n -> p c n", p=128)
                wosrc = P["wo"].rearrange("(c p) n -> p c n", p=128)
                for c in range(8):
                    K.dma("pool", wd[:, c, :], wdsrc[:, c, :], writes=[f"wd{c}"])
                    K.dma("pool", wo[:, c, :], wosrc[:, c, :], writes=[f"wo{c}"])
                K.dma("pool", wsT[:], P["wsT"][:, :])
                K.dma("sp", gmix_bc[:], P["gmix"].partition_broadcast(128))
                K.dma("sp", sgg_bc[:], P["sgg"].partition_broadcast(128))
                K.dma("sp", esink[:], P["sink"].partition_broadcast(128))
                K.dma("sp", gmT[:], P["gmT"][:, :])
                K.dma("sp", cw[:], P["cw"][:, :])
                K.dma("sp", sbT[:], P["sbT"][:, :])
                K.act(esink[:], esink[:], AF.Exp)
                K.op("dve", lambda g: g.memset(zpad[:], 0.0), [], ["zpad"])
                for pc in (0, NLAT + 1, NLAT + 2, NTOK + 3):
                    K.dma("sp", zD[:, :, pc:pc + 1], zpad[:], writes=[f"zp{pc}"], slow=True)
                WDK = [f"wd{c}" for c in range(8)]
                WOK = [f"wo{c}" for c in range(8)]
                ZPK = [f"zp{pc}" for pc in (0, NLAT + 1, NLAT + 2, NTOK + 3)]

                def zoff(b):
                    return 1 + b * 128 + (2 if b >= NKV else 0)

                def m1_group(gi, blks):
                    s = 1 if blks[0] >= NKV else 0
                    par = gi % 2
                    N = 128 * len(blks)
                    tok0 = blks[0] * 128
                    z0 = zoff(blks[0])
                    K.dma("pool", cosg[:, 0:N], cos_in[:, tok0:tok0 + N])
                    K.dma("pool", sing[:, 0:N], sin_in[:, tok0:tok0 + N])
                    for bi, b in enumerate(blks):
                        x_ = xt[bi % 2]
                        K.dma("sp", x_[:], xrows(b))
                        norm_mod(x_[:], hf[:], gm1[s][:], sh1[s][:], validT[:, b:b + 1], 0)
                        transpose_to(hf, 8, lambda c0, n, bi=bi: hTg[:, c0:c0 + n, bi * 128:(bi + 1) * 128])
                        yield

                    def proj(col0, bank):
                        ps = R[bank]
                        for c in range(8):
                            K.mm(ps[:, 0:N], wd[:, c, col0:col0 + 128], hTg[:, c, 0:N], start=(c == 0), stop=(c == 7),
                                 rk=[WDK[c], "hTg"])
                        return ps
                    for j in range(2):
                        ps = proj(C_BG + j * 128, 2)
                        K.cp("act", bgg[:, j, 0:N], ps[:, 0:N])
                        ps = proj(C_CG + j * 128, 3)
                        K.cp("act", cgt[:, 0:N], ps[:, 0:N])
                        ps = proj(C_XI + j * 128, 2)
                        K.tt("dve", zg[:, j, 0:N], ps[:, 0:N], cgt[:, 0:N], ALU.mult)
                        yield
                    K.dma("sp", zD[:, :, z0:z0 + N], zg[:, :, 0:N], writes=[f"z{gi}"])
                    K.dma("sp", bgD[:, :, z0:z0 + N], bgg[:, :, 0:N], writes=[f"bg{gi}"])

                    def rope(ca_, cb_, dst, wk):
                        pa = proj(ca_, 2)
                        pb = proj(cb_, 3)
                        K.tt("dve", tmpA[:, 0:N], pa[:, 0:N], cosg[:, 0:N], ALU.mult)
                        K.tt("dve", tmpB[:, 0:N], pb[:, 0:N], sing[:, 0:N], ALU.mult)
                        K.tt("pool", dst, tmpA[:, 0:N], tmpB[:, 0:N], ALU.add, wk=wk)
                    for c in range(4):
                        rope(C_Q + c * 128, C_QP + c * 128, QT[par][:, c, 0:N], [KN(QT[par])])
                        yield
                    rope(C_K, C_KP, KT[:, tok0:tok0 + N], [f"kt{b_}" for b_ in blks])
                    yield
                    for bi, b in enumerate(blks):
                        tsl = slice(bi * 128, (bi + 1) * 128)
                        ps = R[4]
                        for c in range(8):
                            K.mm(ps[:, :], hTg[:, c, tsl], wd[:, c, C_SGU:C_SGU + 512], start=(c == 0), stop=(c == 7),
                                 rk=[WDK[c], "hTg"])
                        K.act(zs[:], ps[:, :], AF.Gelu_apprx_tanh)
                        yield
                        K.dv("bn_stats", ["zs"], ["bnst"], out=bnst[:, 0:6], in_=zs[:, 256:512])
                        K.dv("bn_aggr", ["bnst"], ["bnst2"], out=bnst[:, 6:8], in_=bnst[:, 0:6])
                        K.act(stat[:, 9:10], bnst[:, 7:8], AF.Ln, bias=EPS, rk=["bnst2"], wk=["st9"])
                        K.act(stat[:, 10:11], stat[:, 9:10], AF.Exp, scale=-0.5, rk=["st9"], wk=["st10"])
                        K.ts(vln[:], zs[:, 256:512], bnst[:, 6:7], stat[:, 10:11], ALU.subtract, ALU.mult,
                             rk=["zs", "bnst2", "st10"])
                        K.tt("dve", vlb[:], vln[:], sgg_bc[:], ALU.mult)
                        p5 = R[5]
                        for hh in range(4):
                            K.mm(p5[:, hh * 64:(hh + 1) * 64], wsT[:, hh * 128:(hh + 1) * 128], vlb[:, hh * 64:(hh + 1) * 64])
                        for hh in range(4):
                            K.stt(ysg[:, hh * 64:(hh + 1) * 64], p5[:, hh * 64:(hh + 1) * 64], sbT[:, hh:hh + 1],
                                  zs[:, hh * 64:(hh + 1) * 64], ALU.add, ALU.mult)
                        K.act(junk[:, 0:256], ysg[:], AF.Square, accum_out=stat[:, 12:13], wk=["junk", "st12"])
                        K.act(stat[:, 13:14], stat[:, 12:13], AF.Ln, bias=EPS, scale=1.0 / 256, rk=["st12"], wk=["st13"])
                        K.act(stat[:, 14:15], stat[:, 13:14], AF.Exp, scale=-0.5, rk=["st13"], wk=["st14"])
                        K.stt(vln[:], ysg[:], stat[:, 14:15], gmix_bc[:, 256:512], ALU.mult, ALU.mult, rk=["ysg", "st14", "gmix_bc"])
                        transpose_to(vln, 2, lambda c0, n, bi=bi: ysgT[par][:, c0:c0 + n, bi * 128:(bi + 1) * 128])
                        yield
                        for c in range(8):
                            K.mm(p5[:, 256:384], hTg[:, c, tsl], wd[:, c, C_V:C_V + 128], start=(c == 0), stop=(c == 7),
                                 rk=[WDK[c], "hTg"])
                        K.cp("act", Va[:, b, :, 0:64], p5[:, 256:384].rearrange("p (g d) -> p g d", g=2), wk=[f"vv{b}"])
                        K.cp("dve", Va[:, b, :, 64:65], validT[:, b:b + 1].unsqueeze(2).to_broadcast([128, 2, 1]),
                             wk=[f"v1{b}"])
                        yield

                def post_block(gi, bi, b):
                    s = 1 if b >= NKV else 0
                    par = gi % 2
                    zo = zoff(b)
                    tsl = slice(bi * 128, (bi + 1) * 128)
                    ZK = [f"z{g_}" for g_ in (gi - 1, gi, gi + 1)] + ZPK
                    K.dma("sp", zb[:], zD[:, :, zo - 1:zo + 129], reads=ZK)
                    K.dma("sp", bgb[:], bgD[:, :, zo:zo + 128], reads=[f"bg{gi}"])
                    K.dma("sp", xr[:], xrows(b))
                    for j in range(2):
                        K.ts(ca[:], zb[:, j, 0:128], cw[:, j * 3:j * 3 + 1], None, ALU.mult)
                        K.stt(ca[:], zb[:, j, 1:129], cw[:, j * 3 + 1:j * 3 + 2], ca[:], ALU.mult, ALU.add)
                        K.stt(ca[:], zb[:, j, 2:130], cw[:, j * 3 + 2:j * 3 + 3], ca[:], ALU.mult, ALU.add)
                        K.tt("dve", yc[:, j, :], ca[:], bgb[:, j, :], ALU.mult)
                    K.tt("pool", ycq[:], yc[:], yc[:], ALU.mult)
                    ps = R[5]
                    for j in range(2):
                        K.mm(ps[:, 384:512], ones_bf[:], ycq[:, j, :], start=(j == 0), stop=(j == 1))
                    K.act(rsb[:], ps[:, 384:512], AF.Ln, bias=EPS, scale=1.0 / 256)
                    K.act(rsb[:], rsb[:], AF.Exp, scale=-0.5)
                    for j in range(2):
                        K.stt(ynT[:, j, :], yc[:, j, :], gmT[:, j:j + 1], rsb[:], ALU.mult, ALU.mult, wk=[f"yn{j}"])
                    yield
                    if b >= NKV:
                        keys = [(NKV, None), (NKV + 1, None)]
                    else:
                        keys = [(b - 1, 0), (b, None), (b + 1, 1), (NKV, None), (NKV + 1, None)]
                    for grp in range(2):
                        rows = slice(grp * 64, grp * 64 + 64)
                        for ki, (kb, m) in enumerate(keys):
                            S = R[2 + ki % 2]
                            K.mm(S[:, :].rearrange("p (c t) -> p c t", c=4), KT[rows, kb * 128:(kb + 1) * 128],
                                 QT[par][rows, :, tsl], rk=[f"kt{kb}", KN(QT[par])])
                            K.act(PT[:, ki, :], S[:, :], AF.Exp, scale=0.125, wk=[f"PT{ki}"])
                            if m is not None:
                                pv = PT[:, ki, :].rearrange("p (c t) -> p c t", c=4)
                                K.tt("pool", pv, pv, masks[:, m * 128:(m + 1) * 128].unsqueeze(1).to_broadcast([128, 4, 128]),
                                     ALU.mult, rk=[f"PT{ki}", "masks"], wk=[f"PT{ki}"])
                        O = R[4]
                        Ov = O[:, :].rearrange("p (c d) -> p c d", c=4)
                        for c in range(4):
                            for ki, (kb, m) in enumerate(keys):
                                K.mm(Ov[:, c, 0:65], PT[:, ki, c * 128:(c + 1) * 128], Va[:, kb, grp, :],
                                     start=(ki == 0), stop=(ki == len(keys) - 1), rk=[f"PT{ki}", f"vv{kb}", f"v1{kb}"])
                        den = stat[:, 16:20]
                        K.tt("dve", den.unsqueeze(2), Ov[:, :, 64:65], esink[:, grp * 4:grp * 4 + 4].unsqueeze(2), ALU.add,
                             wk=["den"])
                        K.dv("reciprocal", ["den"], ["rden"], out=stat[:, 20:24], in_=den)
                        K.tt("dve", att[:, grp * 256:(grp + 1) * 256].rearrange("p (c d) -> p c d", c=4), Ov[:, :, 0:64],
                             stat[:, 20:24].unsqueeze(2).to_broadcast([128, 4, 64]), ALU.mult, rk=[KN(O), "rden"])
                        yield
                    K.act(junk[:, 0:512], att[:], AF.Square, accum_out=stat[:, 24:25], wk=["junk", "st24"])
                    K.act(stat[:, 25:26], stat[:, 24:25], AF.Ln, bias=EPS, scale=1.0 / 512, rk=["st24"], wk=["st25"])
                    K.act(stat[:, 26:27], stat[:, 25:26], AF.Exp, scale=-0.5, rk=["st25"], wk=["st26"])
                    K.stt(att[:], att[:], stat[:, 26:27], gmix_bc[:, 512:1024], ALU.mult, ALU.mult, rk=["att", "st26", "gmix_bc"])
                    transpose_to(att, 4, lambda c0, n: ynT[:, 4 + c0:4 + c0 + n, :])
                    yield
                    for hh in range(2):
                        ps = R[6 + hh]
                        for k in range(8):
                            lhs = ysgT[par][:, k - 2, tsl] if k in (2, 3) else ynT[:, k, :]
                            K.mm(ps[:, :], lhs, wo[:, k, hh * 512:(hh + 1) * 512], start=(k == 0), stop=(k == 7),
                                 rk=[KN(ysgT[par]) if k in (2, 3) else ("ynT" if k >= 4 else f"yn{k}"), WOK[k]])
                        K.tt("dve", xn[:, hh * 512:(hh + 1) * 512], ps[:, :], g1[s][:, hh * 512:(hh + 1) * 512], ALU.mult,
                             wk=[f"xn{hh}"])
                        K.tt("pool", xn[:, hh * 512:(hh + 1) * 512], xn[:, hh * 512:(hh + 1) * 512],
                             xr[:, hh * 512:(hh + 1) * 512], ALU.add, rk=[f"xn{hh}", "xr"], wk=[f"xn{hh}"])
                        yield
                    K.dma("sp", xdst[b * 128:(b + 1) * 128, :], xn[:], reads=["xn0", "xn1"], writes=[f"xm{b}"])

                groups = [[NKV, NKV + 1]]
                bl = list(range(lat_lo, lat_hi))
                for i in range(0, len(bl), 4):
                    groups.append(bl[i:i + 4])
                do_ctx_post = not last

                def post_gens(pg, which):
                    pgi, pbl = pg
                    for bi, b in enumerate(pbl):
                        is_last = (bi == len(pbl) - 1)
                        if which == "early" and is_last:
                            continue
                        if which == "late" and not is_last:
                            continue
                        if b >= NKV:
                            if do_ctx_post:
                                yield from post_block(pgi, bi, b)
                        elif post_lo <= b < post_hi:
                            yield from post_block(pgi, bi, b)
                prev = None
                for gi, blks in enumerate(groups):
                    gm = m1_group(gi, blks)
                    gp = post_gens(prev, "early") if (prev is not None and prev[0] > 0) else None
                    if prev is not None and prev[0] == 0:
                        gp = post_gens(prev, "all")
                    while gm is not None or gp is not None:
                        if gm is not None and next(gm, "done") == "done":
                            gm = None
                        if gp is not None and next(gp, "done") == "done":
                            gp = None
                    if l == 0:
                        conv_pump(0, 5)
                    if prev is not None and prev[0] > 0:
                        for _ in post_gens(prev, "late"):
                            pass
                    prev = (gi, blks)
                for _ in post_gens(prev, "all"):
                    pass
                if l == 0:
                    conv_pump(0, 1000)
                K.barrier()

            if not last:
                pblocks = list(range(post_lo, post_hi)) + [NKV, NKV + 1]
            else:
                pblocks = list(range(post_lo, post_hi))
            with ExitStack() as st:
                gm2 = [sb(f"gm2_{s}", [128, D], BF16, st) for s in range(2)]
                sh2 = [sb(f"sh2_{s}", [128, D], BF16, st) for s in range(2)]
                g2 = [sb(f"g2_{s}", [128, D], BF16, st) for s in range(2)]
                emit_mod(l, [(3, sh2, None), (4, gm2, P["n2g"]), (5, g2, None)])
                UTg = [sb(f"UTg{i}", [128, 8, 512], BF16, st) for i in range(2)]
                Vg = [sb(f"Vg{i}", [128, 4, D], BF16, st) for i in range(2)]
                wqj = [sb(f"wqj{i}", [128, 8, 128], BF16, st) for i in range(2)]
                keysT = sb("keysT", [128, 2048], BF16, st)
                Wsb = sb("Wsb", [128, 256, 128], BF16, st)
                hT2 = [sb(f"hT2_{i}", [128, 8, 256], BF16, st) for i in range(2)]
                qT = sb("qT", [128, 16, 256], BF16, st)
                xin1 = sb("xin1", [128, D], F32, st)
                hf2 = sb("hf2", [128, D], F32, st)
                Ssb = sb("Ssb", [128, 2048], F32, st)
                wrk = sb("wrk", [128, 2048], F32, st)
                sv = sb("sv", [128, 256], F32, st)
                si = sb("si", [128, 256], U32, st)
                sif = sb("sif", [128, 256], F32, st)
                fv = sb("fv", [128, 128], F32, st)
                fi = sb("fi", [128, 128], U32, st)
                fif = sb("fif", [128, 128], F32, st)
                faf = sb("faf", [128, 128], F32, st)
                fbf = sb("fbf", [128, 128], F32, st)
                If = sb("If", [128, 128], F32, st)
                Jf = sb("Jf", [128, 128], F32, st)
                gf = sb("gf", [128, 128], F32, st)
                zz = sb("zz", [128, 16], F32, st)
                ITb = [sb(f"ITb{i}", [128, 256], BF16, st) for i in range(2)]
                JTb = [sb(f"JTb{i}", [128, 256], BF16, st) for i in range(2)]
                gTb = [sb(f"gTb{i}", [128, 256], BF16, st) for i in range(2)]
                iob = sb("iob", [128, 128], BF16, st)
                Lb = [sb(f"Lb{i}", [128, 8, 128], BF16, st) for i in range(2)]
                Rb = [sb(f"Rb{i}", [128, 8, 128], BF16, st) for i in range(2)]
                Gs = [sb(f"Gs{i}", [128, 256], BF16, st) for i in range(3)]
                Hs = [sb(f"Hs{i}", [128, 256], BF16, st) for i in range(4)]
                tmpP = sb("tmpP", [128, 512], F32, st)

                wqsrc = P["wqb"].rearrange("(c p) n -> p c n", p=128)
                K.dma("pool", keysT[:], P["keysT"][:, :])
                K.cp("dve", iob[:], iota128[:])
                utsrc = P["utb"].rearrange("(c p) n -> p c n", p=128)
                vsrc = P["vb"].rearrange("(j i) d -> i j d", i=128)

                def load_group(jg):
                    i = jg % 2
                    K.dma("sp", UTg[i][:], utsrc[:, :, jg * 512:(jg + 1) * 512], reads=[])
                    K.dma("sp", Vg[i][:], vsrc[:, jg * 4:(jg + 1) * 4, :], reads=[])

                def p1_gen(ti, blks):
                    par = ti % 2
                    nb = len(blks)
                    N = nb * 128
                    h2 = hT2[par]
                    for bi, b in enumerate(blks):
                        s = 1 if b >= NKV else 0
                        K.dma("sp", xin1[:], xdst[b * 128:(b + 1) * 128, :])
                        ss, rs = stat[:, 30:31], stat[:, 31:32]
                        K.act(junk[:], xin1[:], AF.Square, accum_out=ss, wk=["junk", "st30"])
                        K.act(rs, ss, AF.Ln, bias=EPS, scale=1.0 / D, rk=["st30"], wk=["st31"])
                        K.act(rs, rs, AF.Exp, scale=-0.5, rk=["st31"], wk=["st31"])
                        K.stt(hf2[:], xin1[:], rs, gm2[s][:], ALU.mult, ALU.mult, rk=["xin1", "st31", KN(gm2[s])])
                        K.tt("dve", hf2[:], hf2[:], sh2[s][:], ALU.add)
                        yield
                        transpose_to(hf2, 8, lambda c0, n, bi=bi: h2[:, c0:c0 + n, bi * 128:(bi + 1) * 128], banks=(4, 4))
                        yield
                    K.dma("sp", wqj[0][:], wqsrc[:, :, 0:128], reads=[])
                    for j in range(16):
                        w = wqj[j % 2]
                        if j + 1 < 16:
                            K.dma("sp", wqj[(j + 1) % 2][:], wqsrc[:, :, (j + 1) * 128:(j + 2) * 128], reads=[])
                        ps = R[4]
                        for c in range(8):
                            K.mm(ps[:, 0:N], w[:, c, :], h2[:, c, 0:N], start=(c == 0), stop=(c == 7))
                        K.cp("act", qT[:, j, 0:N], ps[:, 0:N], wk=[f"qT{j}"])
                        yield
                    for bi, b in enumerate(blks):
                        tsl = slice(bi * 128, (bi + 1) * 128)
                        for q4 in range(4):
                            ps = R[4]
                            for jj in range(4):
                                j = q4 * 4 + jj
                                K.mm(ps[:, jj * 128:(jj + 1) * 128], qT[:, j, tsl], keysT[:, j * 128:(j + 1) * 128],
                                     rk=[f"qT{j}", "keysT"])
                            K.cp("act", Ssb[:, q4 * 512:(q4 + 1) * 512], ps[:, :], wk=[f"S{q4}"])
                            yield

                        def top16_all(n, srcf, wkf, valf, idxf, key, srckeys):
                            for j in range(n):
                                K.dv("max", srckeys(j), [f"{key}{j}v0"], out=valf(j)[:, 0:8], in_=srcf(j))
                            yield
                            for j in range(n):
                                K.dv("max_index", srckeys(j) + [f"{key}{j}v0"], [f"{key}{j}i0"], out=idxf(j)[:, 0:8],
                                     in_max=valf(j)[:, 0:8], in_values=srcf(j))
                            yield
                            for j in range(n):
                                K.dv("match_replace", srckeys(j) + [f"{key}{j}v0"], [f"{key}{j}w"], out=wkf(j),
                                     in_to_replace=valf(j)[:, 0:8], in_values=srcf(j), imm_value=-1e30)
                            yield
                            for j in range(n):
                                K.dv("max", [f"{key}{j}w"], [f"{key}{j}v1"], out=valf(j)[:, 8:16], in_=wkf(j))
                            yield
                            for j in range(n):
                                K.dv("max_index", [f"{key}{j}w", f"{key}{j}v1"], [f"{key}{j}i1"], out=idxf(j)[:, 8:16],
                                     in_max=valf(j)[:, 8:16], in_values=wkf(j))
                            yield
                        yield from top16_all(16, lambda j: Ssb[:, j * 128:(j + 1) * 128], lambda j: wrk[:, j * 128:(j + 1) * 128],
                                             lambda j: sv[:, j * 16:(j + 1) * 16], lambda j: si[:, j * 16:(j + 1) * 16], "t",
                                             lambda j: [f"S{j // 4}"])
                        SVK = [f"t{j}v{u}" for j in range(16) for u in range(2)]
                        SIK = [f"t{j}i{u}" for j in range(16) for u in range(2)]
                        TWK = [f"t{j}w" for j in range(16)]
                        sv4 = sv[:].rearrange("p (h t a) -> p h t a", h=8, t=2)
                        K.tt("dve", Ssb[:].rearrange("p (h a b) -> p h a b", h=8, a=16),
                             sv4[:, :, 0, :].unsqueeze(3).to_broadcast([128, 8, 16, 16]),
                             sv4[:, :, 1, :].unsqueeze(2).to_broadcast([128, 8, 16, 16]), ALU.add, rk=SVK, wk=["cand"])
                        yield from top16_all(8, lambda h: Ssb[:, h * 256:(h + 1) * 256], lambda h: wrk[:, h * 256:(h + 1) * 256],
                                             lambda h: fv[:, h * 16:(h + 1) * 16], lambda h: fi[:, h * 16:(h + 1) * 16], "f",
                                             lambda h: ["cand", "S0", "S1", "S2", "S3"] + TWK)
                        FVK = [f"f{h}v{u}" for h in range(8) for u in range(2)]
                        FIK = [f"f{h}i{u}" for h in range(8) for u in range(2)]
                        FWK = [f"f{h}w" for h in range(8)]
                        fv3 = fv[:].rearrange("p (h k) -> p h k", h=8)
                        gf3 = gf[:].rearrange("p (h k) -> p h k", h=8)
                        K.tt("dve", gf3, fv3, fv3[:, :, 0:1].to_broadcast([128, 8, 16]), ALU.subtract, rk=FVK, wk=["gf"])
                        K.act(gf[:], gf[:], AF.Exp)
                        K.cp("dve", fif[:], fi[:], rk=FIK)
                        K.cp("dve", sif[:], si[:], rk=SIK)
                        yield
                        K.dv("tensor_reduce", ["gf"], ["zz"], out=zz[:, 0:8], in_=gf3, axis=AX.X, op=ALU.add)
                        ge3 = wrk[:, 0:1920].rearrange("p (n m) -> p n m", m=15)
                        K.tt("dve", ge3, fif[:].unsqueeze(2).to_broadcast([128, 128, 15]),
                             thr16[:, 1:16].unsqueeze(1).to_broadcast([128, 128, 15]), ALU.is_ge,
                             rk=["fif", "thr16"] + FWK, wk=["eq"])
                        K.dv("reciprocal", ["zz"], ["zz2"], out=zz[:, 8:16], in_=zz[:, 0:8])
                        K.dv("tensor_reduce", ["eq"], ["faf"], out=faf[:], in_=ge3, axis=AX.X, op=ALU.add)
                        K.tt("dve", gf3, gf3, zz[:, 8:16].unsqueeze(2).to_broadcast([128, 8, 16]), ALU.mult, rk=["gf", "zz2"])
                        K.stt(fbf[:], faf[:], -16.0, fif[:], ALU.mult, ALU.add)
                        yield
                        sif4 = sif[:].rearrange("p (h t a) -> p h t a", h=8, t=2)
                        eq4 = wrk[:].rearrange("p (h k a) -> p h k a", h=8, k=16)
                        io4 = iota16[:].unsqueeze(1).unsqueeze(1).to_broadcast([128, 8, 16, 16])
                        for (ff, t_, dst) in ((faf, 0, If), (fbf, 1, Jf)):
                            K.tt("dve", eq4, io4, ff[:].rearrange("p (h k) -> p h k", h=8).unsqueeze(3).to_broadcast([128, 8, 16, 16]),
                                 ALU.is_equal, rk=["iota16", KN(ff), "faf"], wk=["eq"])
                            K.tt("dve", eq4, eq4, sif4[:, :, t_, :].unsqueeze(2).to_broadcast([128, 8, 16, 16]), ALU.mult,
                                 rk=["eq", "sif"], wk=["eq"])
                            K.dv("tensor_reduce", ["eq"], [KN(dst)], out=dst[:].rearrange("p (h k) -> p h k", h=8), in_=eq4,
                                 axis=AX.X, op=ALU.add)
                            yield
                        ps = R[4]
                        K.tr(ps[:, 0:128], If[:], ident[:])
                        K.tr(ps[:, 128:256], Jf[:], ident[:])
                        K.tr(ps[:, 256:384], gf[:], ident[:])
                        K.cp("act", ITb[par][:, tsl], ps[:, 0:128], wk=[f"IT{par}{bi}"])
                        K.cp("act", JTb[par][:, tsl], ps[:, 128:256], wk=[f"JT{par}{bi}"])
                        K.cp("act", gTb[par][:, tsl], ps[:, 256:384], wk=[f"gT{par}{bi}"])
                        yield

                def p2(ti, blks):
                    par = ti % 2
                    N = len(blks) * 128
                    for gidx, t0 in enumerate(range(0, N, 4)):
                        bi = t0 // 128
                        pp = gidx % 4
                        for u in range(4):
                            t = t0 + u
                            K.ts(Lb[pp // 2][:, (pp % 2) * 4 + u, :], iob[:], ITb[par][:, t:t + 1], gTb[par][:, t:t + 1],
                                 ALU.is_equal, ALU.mult, rk=["iob", f"IT{par}{bi}", f"gT{par}{bi}"], wk=[f"L{pp}"])
                            K.ts(Rb[pp // 2][:, (pp % 2) * 4 + u, :], iob[:], JTb[par][:, t:t + 1], None,
                                 ALU.is_equal, rk=["iob", f"JT{par}{bi}"], wk=[f"Rr{pp}"])
                        ps = R[4 + gidx % 4]
                        for u in range(4):
                            K.mm(ps[:, u * 128:(u + 1) * 128], Lb[pp // 2][:, (pp % 2) * 4 + u, :], Rb[pp // 2][:, (pp % 2) * 4 + u, :],
                                 rk=[f"L{pp}", f"Rr{pp}"])
                        K.cp("act", Wsb[:, t0:t0 + 4, :], ps[:, :].rearrange("p (t j) -> p t j", t=4), wk=["Wsb"])

                def p3_gen(ti, blks):
                    par = ti % 2
                    nb = len(blks)
                    N = nb * 128
                    h2 = hT2[par]

                    def emitA(j):
                        i, jj = (j // 4) % 2, j % 4
                        A = R[5 + j % 3][:, 0:N]
                        for c in range(8):
                            K.mm(A, UTg[i][:, c, jj * 128:(jj + 1) * 128], h2[:, c, 0:N], start=(c == 0), stop=(c == 7))
                    load_group(0)
                    load_group(1)
                    emitA(0)
                    emitA(1)
                    for j in range(128):
                        jg, jj = j // 4, j % 4
                        i = jg % 2
                        if j + 2 < 128:
                            emitA(j + 2)
                        A = R[5 + j % 3][:, 0:N]
                        G = Gs[j % 3]
                        K.act(G[:, 0:N], A, AF.Gelu_apprx_tanh)
                        H = Hs[j % 4]
                        K.tt("pool", H[:, 0:N], G[:, 0:N], Wsb[:, 0:N, j], ALU.mult)
                        for bi in range(nb):
                            for hh in range(2):
                                K.mm(R[bi * 2 + hh][:, :], H[:, bi * 128:(bi + 1) * 128], Vg[i][:, jj, hh * 512:(hh + 1) * 512],
                                     start=(j == 0), stop=(j == 127))
                        if jj == 3 and jg + 2 < 32:
                            load_group(jg + 2)
                        yield

                def p4(ti, blks):
                    for bi, b in enumerate(blks):
                        s = 1 if b >= NKV else 0
                        K.dma("sp", xin1[:], xdst[b * 128:(b + 1) * 128, :])
                        for hh in range(2):
                            hs = slice(hh * 512, (hh + 1) * 512)
                            K.tt("dve", tmpP[:], R[bi * 2 + hh][:, :], g2[s][:, hs], ALU.mult)
                            K.tt("pool", hf2[:, hs], tmpP[:], xin1[:, hs], ALU.add, rk=["tmpP", "xin1"], wk=["hf2"])
                        if last:
                            ss, rs = stat[:, 34:35], stat[:, 35:36]
                            K.act(junk[:], hf2[:], AF.Square, accum_out=ss, rk=["hf2"], wk=["junk", "st34"])
                            K.act(rs, ss, AF.Ln, bias=EPS, scale=1.0 / D, rk=["st34"], wk=["st35"])
                            K.act(rs, rs, AF.Exp, scale=-0.5, rk=["st35"], wk=["st35"])
                            K.stt(hf2[:], hf2[:], rs, fg_bc[:], ALU.mult, ALU.mult, rk=["hf2", "st35", "fg_bc"], wk=["hf2"])
                            ob = b - post_lo
                            K.dma("sp", out_d[ob * 128:(ob + 1) * 128, :], hf2[:], reads=["hf2"], writes=[f"o{b}"])
                        else:
                            K.dma("sp", x1[b * 128:(b + 1) * 128, :], hf2[:], reads=["hf2"], writes=[f"x1_{b}"])

                tiles = [pblocks[i_:i_ + 2] for i_ in range(0, len(pblocks), 2)]
                for _ in p1_gen(0, tiles[0]):
                    pass
                for ti, blks in enumerate(tiles):
                    p2(ti, blks)
                    g1 = p1_gen(ti + 1, tiles[ti + 1]) if ti + 1 < len(tiles) else None
                    for step, _ in enumerate(p3_gen(ti, blks)):
                        if g1 is not None and step % 2 == 1:
                            if next(g1, "done") == "done":
                                g1 = None
                    if g1 is not None:
                        for _ in g1:
                            pass
                    p4(ti, blks)
                    if l + 1 < DEPTH:
                        conv_pump(l + 1, 3)
                if l + 1 < DEPTH:
                    conv_pump(l + 1, 1000)
                K.barrier()
        K.barrier()
        print("instruction counts:", K.ninst, "sem incs:", K.ninc)
        waited = K.waited
    return nc, waited


def _host_inputs(NB, x, c, ctx, c_ctx, w_ada, b_ada, norm1_g, norm2_g, w_in, conv_w, sgu_norm_g, sgu_w, sgu_b,
                 attn_sink, mix_norm_g, w_out, peer_wq, peer_keys, peer_u, peer_v, final_g):
    f32 = np.float32
    B, S, _ = x.shape
    HALF = NB * 128
    assert S == 2 * HALF
    NKV = NB + 4
    NLAT = NKV * 128
    NTOK = NLAT + 256
    NBLK = NKV + 2
    DEPTH = w_in.shape[0]
    cols = list(range(0, 1280)) + list(range(1920, 2048))
    qcols = []
    for cch in range(4):
        for h in (cch, 4 + cch):
            qcols += [1280 + h * 64 + d for d in range(64)]
    kcols = [1792 + i for i in range(128)]

    def perm(d):
        return d + 16 if (d % 32) < 16 else d - 16
    qpcols = [1280 + ((q - 1280) // 64) * 64 + perm((q - 1280) % 64) for q in qcols]
    kpcols = [1792 + ((k - 1792) // 64) * 64 + perm((k - 1792) % 64) for k in kcols]
    colidx = np.array(cols + qcols + kcols + qpcols + kpcols)
    assert len(colidx) == NCOLD
    shared = {}
    shared["ident"] = np.eye(128, dtype=f32)
    shared["iota128"] = np.tile(np.arange(128, dtype=f32)[None, :], (128, 1))
    shared["iota16"] = np.tile(np.arange(16, dtype=f32)[None, :], (128, 1))
    kk = np.arange(128)[:, None]
    qq = np.arange(128)[None, :]
    shared["masks"] = np.concatenate([(kk >= qq), (kk <= qq)], axis=1).astype(f32)
    shared["final_g"] = np.ascontiguousarray(final_g.reshape(1, D), dtype=f32)
    for l in range(DEPTH):
        shared[f"wada{l}"] = np.ascontiguousarray(w_ada[l])
        shared[f"bada{l}"] = np.ascontiguousarray(b_ada[l].reshape(1, -1))
        shared[f"n1g{l}"] = np.ascontiguousarray(norm1_g[l].reshape(1, -1))
        shared[f"n2g{l}"] = np.ascontiguousarray(norm2_g[l].reshape(1, -1))
        shared[f"wd{l}"] = np.ascontiguousarray(w_in[l][:, colidx])
        shared[f"cw{l}"] = np.ascontiguousarray(conv_w[l].reshape(3, 2, 128).transpose(2, 1, 0).reshape(128, 6))
        shared[f"sgg{l}"] = np.ascontiguousarray(sgu_norm_g[l].reshape(1, -1))
        shared[f"wsT{l}"] = np.ascontiguousarray(sgu_w[l].transpose(2, 0, 1).reshape(128, 512))
        shared[f"sbT{l}"] = np.ascontiguousarray(sgu_b[l].T)
        shared[f"sink{l}"] = np.ascontiguousarray(attn_sink[l].reshape(1, 8))
        shared[f"gmix{l}"] = np.ascontiguousarray(mix_norm_g[l].reshape(1, -1))
        shared[f"gmT{l}"] = np.ascontiguousarray(mix_norm_g[l].reshape(8, 128).T)
        shared[f"wo{l}"] = np.ascontiguousarray(w_out[l])
        shared[f"wq{l}"] = np.ascontiguousarray(peer_wq[l])
        shared[f"keysT{l}"] = np.ascontiguousarray(peer_keys[l].reshape(16, 128, 128).transpose(2, 0, 1).reshape(128, 2048))
        shared[f"ut{l}"] = np.ascontiguousarray(peer_u[l].reshape(128, 128, D).transpose(2, 1, 0).reshape(D, 16384))
        shared[f"vp{l}"] = np.ascontiguousarray(peer_v[l].reshape(128, 128, D).transpose(1, 0, 2).reshape(16384, D))
    inv = (10000.0 ** (-np.arange(16, dtype=np.float32) / 16)).astype(f32)
    sign = np.where((np.arange(64) % 32) < 16, -1.0, 1.0).astype(f32)
    in_maps = []
    for core in range(2 * B):
        b, hf = core // 2, core % 2
        start = hf * HALF
        m = dict(shared)
        tpos = np.arange(start - 256, start + HALF + 256)
        valid = ((tpos >= 0) & (tpos < S))
        xe = np.zeros((NLAT, D), f32)
        xe[valid] = x[b, tpos[valid]]
        m["x_ext"] = xe
        m["ctx_in"] = np.ascontiguousarray(ctx[b])
        cT = np.zeros((128, 8, 2), f32)
        cT[:, :, 0] = c[b].reshape(8, 128).T
        cT[:, :, 1] = c_ctx.reshape(8, 128).T
        m["cT"] = cT.reshape(128, 16)
        vfull = np.concatenate([valid.astype(f32), np.ones(256, f32)])
        m["validT"] = np.ascontiguousarray(vfull.reshape(NBLK, 128).T)
        tp = np.clip(tpos, 0, S - 1)
        row = (tp // 64).astype(f32)
        col = (tp % 64).astype(f32)
        ar = row[:, None] * inv[None, :]
        ac = col[:, None] * inv[None, :]
        ang = np.concatenate([ar, ar, ac, ac], axis=-1).astype(f32)
        cos = np.concatenate([np.cos(ang), np.ones((256, 64), f32)], axis=0)
        sin = np.concatenate([np.sin(ang) * sign[None, :], np.zeros((256, 64), f32)], axis=0)
        m["cosT"] = np.ascontiguousarray(np.tile(cos.T, (2, 1)), dtype=f32)
        m["sinT"] = np.ascontiguousarray(np.tile(sin.T, (2, 1)), dtype=f32)
        in_maps.append(m)
    return in_maps


_CACHE = {}


def kernel(**inputs):
    inputs = {k: np.asarray(v) for k, v in inputs.items()}
    x = inputs["x"]
    B, S, _ = x.shape
    NB = S // 256
    if NB not in _CACHE:
        _CACHE[NB] = build_program(NB, DEPTH=inputs["w_in"].shape[0])
    nc = _CACHE[NB]
    in_maps = _host_inputs(NB, **inputs)
    res = run_bass_kernel_spmd(nc, in_maps, core_ids=list(range(2 * B)))
    out = np.zeros((B, S, D), np.float32)
    for core in range(2 * B):
        b, hf = core // 2, core % 2
        out[b, hf * NB * 128:(hf + 1) * NB * 128] = res.results[core]["out"]
    return out
```
